# Optimizing a Trainium2 kernel written in Bass

```python
import jax, jax.numpy as jnp
from jax import lax
import numpy as np

D_MODEL = 2048
BATCH = 1
SEQ = 8192
DEPTH = 1

GRID_W = 64
CTX_LEN = 256
MIX_WIDTH = D_MODEL
POOL_WIDTH = MIX_WIDTH // 2
POOL_WINDOWS = (2, 4, 8, 16)
N_POOL_GROUPS = len(POOL_WINDOWS)
POOL_GROUP = POOL_WIDTH // N_POOL_GROUPS
MLSTM_WIDTH = MIX_WIDTH - POOL_WIDTH
MLSTM_HEADS = 4
MLSTM_HEAD_DIM = MLSTM_WIDTH // MLSTM_HEADS
CHUNK = 64
QK_CONV = 3
N_GROUPS = 4
EXPERTS_PER_GROUP = 8
N_EXPERTS = N_GROUPS * EXPERTS_PER_GROUP
TOP_K = 2
D_EXPERT = 512
EPS = 1e-6
Q0 = POOL_WIDTH
O0 = Q0 + MLSTM_WIDTH
K0 = O0 + MLSTM_WIDTH
V0 = K0 + MLSTM_WIDTH
G0 = V0 + MLSTM_WIDTH
IN_COLS = G0 + 4 * MLSTM_HEADS

kernel_name = "hymba_pool_mlstm_hmoe_dit_layer"


def rmsnorm(x, g):
    xf = x.astype(jnp.float32)
    y = xf * lax.rsqrt(jnp.mean(xf * xf, axis=-1, keepdims=True) + EPS)
    return (y * g).astype(x.dtype)


def modulate(h, shift, scale):
    return h * (1.0 + scale) + shift


def adaln(cond, w, b):
    return jax.nn.silu(cond) @ w + b


def box_mean(x, win, axis):
    L = x.shape[axis]
    t = np.arange(L)
    lo = np.clip(t - win // 2, 0, L)
    hi = np.clip(t - win // 2 + win, 0, L)
    xf = jnp.moveaxis(x.astype(jnp.float32), axis, 0)
    cs = jnp.concatenate([jnp.zeros_like(xf[:1]), jnp.cumsum(xf, axis=0)], axis=0)
    cnt = jnp.asarray(hi - lo, jnp.float32).reshape((L,) + (1,) * (xf.ndim - 1))
    return jnp.moveaxis((cs[hi] - cs[lo]) / cnt, 0, axis)


def pool_mix(u, w_pool, pool_scale, rows):
    B, T, _ = u.shape
    ug = u.reshape(B, T, N_POOL_GROUPS, POOL_GROUP)
    diffs = []
    for gi, win in enumerate(POOL_WINDOWS):
        ux = ug[:, :, gi]
        if rows is None:
            mean = box_mean(ux, win, 1)
        else:
            grid = ux.reshape(B, rows, GRID_W, POOL_GROUP)
            mean = box_mean(box_mean(grid, win, 1), win, 2).reshape(B, T, POOL_GROUP)
        diffs.append(mean.astype(u.dtype) - ux)
    d = jnp.stack(diffs, axis=2)
    y = jnp.einsum("btgc,gcd->btgd", d, w_pool).reshape(B, T, POOL_WIDTH)
    return y * pool_scale


def dwconv(x, w):
    K, C = w.shape
    return lax.conv_general_dilated(x, w[:, None, :].astype(x.dtype), (1,), [(K // 2, K // 2)],
                                    dimension_numbers=("NWC", "WIO", "NWC"), feature_group_count=C)


def split_heads(a):
    B, T, _ = a.shape
    return a.reshape(B, T, MLSTM_HEADS, MLSTM_HEAD_DIM).transpose(0, 2, 1, 3)


def mlstm_q(u_q, w_conv_q):
    return split_heads(jax.nn.silu(dwconv(u_q, w_conv_q)) * (MLSTM_HEAD_DIM ** -0.5))


def mlstm_kv_gates(p, w_conv_k, gate_bias):
    B, T, _ = p.shape
    k = jax.nn.silu(dwconv(p[..., :MLSTM_WIDTH], w_conv_k))
    v = p[..., MLSTM_WIDTH:2 * MLSTM_WIDTH]
    gates = p[..., 2 * MLSTM_WIDTH:].astype(jnp.float32) + gate_bias.reshape(-1).astype(jnp.float32)
    gates = gates.reshape(B, T, 4, MLSTM_HEADS).transpose(2, 0, 3, 1)
    return split_heads(k), split_heads(v), gates


def zero_state(B):
    H, Dh = MLSTM_HEADS, MLSTM_HEAD_DIM
    return (jnp.zeros((B, H, Dh, Dh), jnp.float32), jnp.zeros((B, H, Dh), jnp.float32),
            jnp.zeros((B, H), jnp.float32))


def mlstm_scan(q, k, v, i_pre, f_pre, state):
    need_out = q is not None
    B, H, T, Dh = k.shape
    nc = T // CHUNK

    def chunks(a):
        a = a.reshape((B, H, nc, CHUNK) + a.shape[3:])
        return jnp.moveaxis(a, 2, 0)

    logf = jax.nn.log_sigmoid(f_pre)
    xs = (chunks(k.astype(jnp.float32)), chunks(v.astype(jnp.float32)), chunks(i_pre), chunks(logf))
    if need_out:
        xs = xs + (chunks(q.astype(jnp.float32)),)
    tril = jnp.tril(jnp.ones((CHUNK, CHUNK), dtype=bool))

    def step(carry, inp):
        C, n, m = carry
        kc, vc, ic, lfc = inp[:4]
        b = jnp.cumsum(lfc, axis=-1)
        g = b[..., -1]
        a = g[..., None] - b + ic
        m_new = jnp.maximum(g + m, jnp.max(a, axis=-1))
        decay = jnp.exp(g + m - m_new)
        wk = jnp.exp(a - m_new[..., None])
        C_new = decay[..., None, None] * C + jnp.einsum("bhl,bhlv,bhlk->bhvk", wk, vc, kc)
        n_new = decay[..., None] * n + jnp.einsum("bhl,bhlk->bhk", wk, kc)
        if not need_out:
            return (C_new, n_new, m_new), None
        qc = inp[4]
        dmat = b[..., :, None] - b[..., None, :] + ic[..., None, :]
        dmat = jnp.where(tril, dmat, -jnp.inf)
        inter = b + m[..., None]
        m_t = jnp.maximum(inter, jnp.max(dmat, axis=-1))
        w_inter = jnp.exp(inter - m_t)
        wts = jnp.exp(dmat - m_t[..., None]) * jnp.einsum("bhtd,bhsd->bhts", qc, kc)
        num = (w_inter[..., None] * jnp.einsum("bhvk,bhtk->bhtv", C, qc)
               + jnp.einsum("bhts,bhsv->bhtv", wts, vc))
        den = w_inter * jnp.einsum("bhk,bhtk->bht", n, qc) + jnp.sum(wts, axis=-1)
        h = num / jnp.maximum(jnp.abs(den), jnp.exp(-m_t))[..., None]
        return (C_new, n_new, m_new), h

    state, hs = lax.scan(step, state, xs)
    if not need_out:
        return state, None
    return state, jnp.moveaxis(hs, 0, 2).reshape(B, H, T, Dh)


def bidir_mlstm(q, k, v, gates, init_f, init_b):
    rev = lambda a: jnp.flip(a, axis=2)
    st_f, h_f = mlstm_scan(q, k, v, gates[0], gates[1], init_f)
    st_b, h_b = mlstm_scan(None if q is None else rev(q), rev(k), rev(v), rev(gates[2]), rev(gates[3]), init_b)
    h = None if q is None else h_f + rev(h_b)
    return st_f, st_b, h


def mlstm_readout(h, o_pre, head_g):
    B, H, T, Dh = h.shape
    mu = jnp.mean(h, axis=-1, keepdims=True)
    var = jnp.mean(jnp.square(h - mu), axis=-1, keepdims=True)
    hn = ((h - mu) * lax.rsqrt(var + EPS)).transpose(0, 2, 1, 3).reshape(B, T, H * Dh)
    return (hn * head_g).astype(o_pre.dtype) * jax.nn.sigmoid(o_pre)


def mix_out(u, h_m, w_pool, pool_scale, head_g, w_out, rows):
    p = pool_mix(u[..., :POOL_WIDTH], w_pool, pool_scale, rows)
    m = mlstm_readout(h_m, u[..., O0:K0], head_g)
    return jnp.concatenate([p, m], axis=-1) @ w_out


def hier_moe(xn, w_group, b_group, w_router, b_router, w1, w3, w2):
    B, T, D = xn.shape
    xt = xn.reshape(B * T, D)
    grp_logits = (xt @ w_group).astype(jnp.float32) + b_group
    p_grp = jax.nn.softmax(grp_logits, axis=-1)
    _, g_sel = lax.top_k(grp_logits, 1)
    exp_logits = ((xt @ w_router).astype(jnp.float32) + b_router).reshape(-1, N_GROUPS, EXPERTS_PER_GROUP)
    sel_logits = jnp.take_along_axis(exp_logits, g_sel[:, :, None], axis=1)[:, 0]
    p_exp = jax.nn.softmax(sel_logits, axis=-1)
    top_p, top_i = lax.top_k(p_exp, TOP_K)
    top_p = top_p / jnp.sum(top_p, axis=-1, keepdims=True)
    w_tok = jnp.take_along_axis(p_grp, g_sel, axis=1) * top_p
    eid = g_sel * EXPERTS_PER_GROUP + top_i
    combine = jnp.einsum("nk,nke->ne", w_tok, jax.nn.one_hot(eid, N_EXPERTS, dtype=jnp.float32)).astype(xt.dtype)
    y = jnp.zeros_like(xt)
    for e in range(N_EXPERTS):
        he = jax.nn.silu(xt @ w1[e]) * (xt @ w3[e])
        y = y + combine[:, e:e + 1] * (he @ w2[e])
    return y.reshape(B, T, D)


def setup_inputs(seed: int = 0) -> dict:
    key = jax.random.key(seed)
    ks = jax.random.split(key, 24)
    nrm = lambda k, s: jax.random.normal(k, s, jnp.float32)
    D = D_MODEL
    fb = jnp.linspace(3.0, 6.0, MLSTM_HEADS)
    zb = jnp.zeros((MLSTM_HEADS,), jnp.float32)
    gate_base = jnp.stack([zb, fb, zb, fb])
    return {
        "x": nrm(ks[0], (BATCH, SEQ, D)),
        "c": nrm(ks[1], (BATCH, D)),
        "ctx": nrm(ks[2], (BATCH, CTX_LEN, D)),
        "c_ctx": nrm(ks[3], (D,)),
        "w_mod": nrm(ks[4], (DEPTH, D, 6 * D)) * (0.5 * D ** -0.5),
        "b_mod": 0.02 * nrm(ks[5], (DEPTH, 6 * D)),
        "norm1_g": 1.0 + 0.02 * nrm(ks[6], (DEPTH, D)),
        "w_in": nrm(ks[7], (DEPTH, D, IN_COLS)) * D ** -0.5,
        "w_conv_q": nrm(ks[8], (DEPTH, QK_CONV, MLSTM_WIDTH)) * QK_CONV ** -0.5,
        "w_conv_k": nrm(ks[9], (DEPTH, QK_CONV, MLSTM_WIDTH)) * QK_CONV ** -0.5,
        "gate_bias": gate_base + 0.1 * nrm(ks[10], (DEPTH, 4, MLSTM_HEADS)),
        "head_norm_g": 1.0 + 0.02 * nrm(ks[11], (DEPTH, MLSTM_WIDTH)),
        "w_pool": nrm(ks[12], (DEPTH, N_POOL_GROUPS, POOL_GROUP, POOL_GROUP)) * POOL_GROUP ** -0.5,
        "pool_scale": 1.0 + 0.02 * nrm(ks[13], (DEPTH, POOL_WIDTH)),
        "w_out": nrm(ks[14], (DEPTH, MIX_WIDTH, D)) * MIX_WIDTH ** -0.5,
        "norm2_g": 1.0 + 0.02 * nrm(ks[15], (DEPTH, D)),
        "w_group": nrm(ks[16], (DEPTH, D, N_GROUPS)) * D ** -0.5,
        "b_group": 0.01 * nrm(ks[17], (DEPTH, N_GROUPS)),
        "w_router": nrm(ks[18], (DEPTH, D, N_EXPERTS)) * D ** -0.5,
        "b_router": 0.01 * nrm(ks[19], (DEPTH, N_EXPERTS)),
        "w1": nrm(ks[20], (DEPTH, N_EXPERTS, D, D_EXPERT)) * D ** -0.5,
        "w3": nrm(ks[21], (DEPTH, N_EXPERTS, D, D_EXPERT)) * D ** -0.5,
        "w2": nrm(ks[22], (DEPTH, N_EXPERTS, D_EXPERT, D)) * D_EXPERT ** -0.5,
        "final_g": 1.0 + 0.02 * nrm(ks[23], (D,)),
    }


def reference(x, c, ctx, c_ctx, w_mod, b_mod, norm1_g, w_in, w_conv_q, w_conv_k, gate_bias,
              head_norm_g, w_pool, pool_scale, w_out, norm2_g, w_group, b_group, w_router,
              b_router, w1, w3, w2, final_g):
    B = x.shape[0]
    rows = x.shape[1] // GRID_W
    D = D_MODEL
    h_lat, h_ctx = x, ctx
    for layer in range(DEPTH):
        last = layer == DEPTH - 1
        sh1, sc1, g1, sh2, sc2, g2 = jnp.split(adaln(c, w_mod[layer], b_mod[layer])[:, None, :], 6, axis=-1)
        n_ctx_mod = 2 if last else 6
        mod_ctx = jnp.split(adaln(c_ctx, w_mod[layer][:, :n_ctx_mod * D], b_mod[layer][:n_ctx_mod * D]),
                            n_ctx_mod, axis=-1)

        cn = modulate(rmsnorm(h_ctx, norm1_g[layer]), mod_ctx[0], mod_ctx[1])
        if last:
            uc_state = cn @ w_in[layer][:, K0:]
            q_c = None
        else:
            uc = cn @ w_in[layer]
            uc_state = uc[..., K0:]
            q_c = mlstm_q(uc[..., Q0:O0], w_conv_q[layer])
        k_c, v_c, gt_c = mlstm_kv_gates(uc_state, w_conv_k[layer], gate_bias[layer])
        st_f, st_b, hm_c = bidir_mlstm(q_c, k_c, v_c, gt_c, zero_state(B), zero_state(B))

        xn = modulate(rmsnorm(h_lat, norm1_g[layer]), sh1, sc1)
        u = xn @ w_in[layer]
        q_l = mlstm_q(u[..., Q0:O0], w_conv_q[layer])
        k_l, v_l, gt_l = mlstm_kv_gates(u[..., K0:], w_conv_k[layer], gate_bias[layer])
        _, _, hm_l = bidir_mlstm(q_l, k_l, v_l, gt_l, st_f, st_b)
        mix_l = mix_out(u, hm_l, w_pool[layer], pool_scale[layer], head_norm_g[layer], w_out[layer], rows)
        h_lat = h_lat + g1 * mix_l
        fn = modulate(rmsnorm(h_lat, norm2_g[layer]), sh2, sc2)
        h_lat = h_lat + g2 * hier_moe(fn, w_group[layer], b_group[layer], w_router[layer],
                                      b_router[layer], w1[layer], w3[layer], w2[layer])

        if not last:
            c_sh1, c_sc1, c_g1, c_sh2, c_sc2, c_g2 = mod_ctx
            mix_c = mix_out(uc, hm_c, w_pool[layer], pool_scale[layer], head_norm_g[layer], w_out[layer], None)
            h_ctx = h_ctx + c_g1 * mix_c
            fc = modulate(rmsnorm(h_ctx, norm2_g[layer]), c_sh2, c_sc2)
            h_ctx = h_ctx + c_g2 * hier_moe(fc, w_group[layer], b_group[layer], w_router[layer],
                                            b_router[layer], w1[layer], w3[layer], w2[layer])
    return rmsnorm(h_lat, final_g)
```

```python
import numpy as np
import concourse.bass as bass
import concourse.mybir as mybir
from concourse.bass_utils import run_bass_kernel_spmd

F32 = mybir.dt.float32
BF16 = mybir.dt.bfloat16
AF = mybir.ActivationFunctionType
ALU = mybir.AluOpType
AX = mybir.AxisListType

D = 2048
NKC = 16
SEQ = 8192
NCORES = 8
TOK = 1024
NT = 8
CTX = 256
POOLW = 1024
Q0, O0, K0, V0, G0 = 1024, 2048, 3072, 4096, 5120
INCOLS = 5136
NE = 32
DE = 512
EPS = 1e-6
WINS = (2, 4, 8, 16)


class Buf:
    __slots__ = ("w", "r", "name")

    def __init__(self, name=""):
        self.w = None
        self.r = {}
        self.name = name


class Tk:
    def __init__(self, nc):
        self.nc = nc
        self.engs = {"pe": nc.tensor, "act": nc.scalar, "dve": nc.vector, "pool": nc.gpsimd, "sp": nc.sync}
        self.semh = {}
        self.cnt = {}
        self.seen = {e: {} for e in self.engs}
        for e in self.engs:
            self.semh[e] = nc.alloc_semaphore("s_" + e)
            self.cnt[e] = 0
        self.pending_sig = {e: False for e in self.engs}
        self.nwaits = 0
        self.nops = 0

        self.dpool = [nc.alloc_semaphore("d_%d" % i) for i in range(40)]
        self.ndp = 0

    def all_sems(self):
        return [self.semh[e] for e in self.engs] + list(self.dpool)

    def dsem(self, name):
        self.semh[name] = self.dpool[self.ndp]
        self.ndp += 1
        self.cnt[name] = 0
        return name

    def _wait(self, eng, k, v):
        if self.seen[eng].get(k, 0) >= v:
            return
        self.engs[eng].wait_ge(self.semh[k], v)
        self.seen[eng][k] = v
        self.nwaits += 1

    def op(self, eng, fn, reads=(), writes=(), dsem=None, sig=True):
        deps = {}

        def add(t):
            if t is None:
                return
            k, v = t
            if deps.get(k, 0) < v:
                deps[k] = v

        for b in reads:
            add(b.w)
        for b in writes:
            add(b.w)
            for k, v in b.r.items():
                add((k, v))
        for k, v in deps.items():
            if k == eng and eng == "pe":
                continue
            self._wait(eng, k, v)
        ins = fn(self.engs[eng])
        self.nops += 1
        if dsem is not None:
            self.cnt[dsem] += 16
            t = (dsem, self.cnt[dsem])
            ins.then_inc(self.semh[dsem], 16)
        elif sig:
            self.cnt[eng] += 1
            t = (eng, self.cnt[eng])
            ins.then_inc(self.semh[eng], 1)
        else:
            t = (eng, self.cnt[eng] + 1)
        for b in writes:
            b.w = t
            b.r = {}
        for b in reads:
            k, v = t
            if b.r.get(k, 0) < v:
                b.r[k] = v
        return ins

    def barrier(self):
        for e in self.engs:
            for k, v in self.cnt.items():
                if v > 0 and not (k == e):
                    self._wait(e, k, v)

    def final_wait(self, eng, keys):
        for k in keys:
            self._wait(eng, k, self.cnt[k])


class Arena:
    def __init__(self, base, nwords):
        self.base = base
        self.n = nwords
        self.free_list = [(0, nwords)]
        self.groups = {}
        self.cur = "g0"
        self.used = 0
        self.peak = 0

    def group(self, name):
        self.cur = name

    def free_group(self, name):
        for iv in self.groups.pop(name, []):
            self.free_list.append(iv)
            self.used -= iv[1]
        self.free_list.sort()
        merged = []
        for st, ln in self.free_list:
            if merged and merged[-1][0] + merged[-1][1] == st:
                merged[-1] = (merged[-1][0], merged[-1][1] + ln)
            else:
                merged.append((st, ln))
        self.free_list = merged

    def alloc(self, nelem, dtype=F32, shape=None):
        words = nelem if dtype == F32 else (nelem + 1) // 2
        words = (words + 15) // 16 * 16
        for idx, (st, ln) in enumerate(self.free_list):
            if ln >= words:
                break
        else:
            raise RuntimeError("arena overflow: need %d words, used %d, free %s" % (words, self.used, self.free_list))
        if ln == words:
            self.free_list.pop(idx)
        else:
            self.free_list[idx] = (st + words, ln - words)
        self.groups.setdefault(self.cur, []).append((st, words))
        self.used += words
        self.peak = max(self.peak, self.used)
        ap = self.base[:, st:st + words]
        if dtype != F32:
            ap = ap.bitcast(dtype)
        ap = ap[:, 0:nelem]
        if shape is not None:
            if len(shape) == 1:
                ap = ap.rearrange("p (a b) -> p a b", a=shape[0])
            elif len(shape) == 2:
                ap = ap.rearrange("p (a b c) -> p a b c", a=shape[0], b=shape[1])
            elif len(shape) == 3:
                ap = ap.rearrange("p (a b c d) -> p a b c d", a=shape[0], b=shape[1], c=shape[2])
        return ap


def build(stop="end", dumps=()):
    nc = bass.Bass("TRN2", target_bir_lowering=False)

    def din(name, shape):
        return nc.dram_tensor(name, list(shape), F32, kind="ExternalInput").ap()

    w_mod = din("w_mod", [D, 6 * D])
    bmod_fm = din("bmod_fm", [128, 96 * 2])
    c_fm = din("c_fm", [128, 32])
    n1g_fm = din("n1g_fm", [128, 32])
    n2g_fm = din("n2g_fm", [128, 16])
    w_in = din("w_in", [D, INCOLS])
    wcq_fm = din("wcq_fm", [128, 24])
    gbias_bc = din("gbias_bc", [128, 16])
    hng_fm = din("hng_fm", [128, 8])
    pscale_fm = din("pscale_fm", [128, 8])
    w_pool = din("w_pool", [4, 256, 256])
    w_out = din("w_out", [D, D])
    wgr_fm = din("wgr_fm", [128, 16 * 36])
    bgr_bc = din("bgr_bc", [128, 36])
    NEd = NE if stop == "end" else 1
    NSPL = 4 if stop == "end" else 1
    EPS_ = NEd // NSPL
    w1s = [din("w1_%d" % j, [EPS_, D, DE]) for j in range(NSPL)]
    w3s = [din("w3_%d" % j, [EPS_, D, DE]) for j in range(NSPL)]
    w2s = [din("w2_%d" % j, [EPS_, DE, D]) for j in range(NSPL)]
    w1 = lambda e: w1s[e // EPS_][e % EPS_]
    w3 = lambda e: w3s[e // EPS_][e % EPS_]
    w2 = lambda e: w2s[e // EPS_][e % EPS_]
    fg_bc = din("fg_bc", [128, D])
    xown = din("xown", [TOK, D])
    xhalo = din("xhalo", [TOK, D])
    xo = din("xo", [7 * TOK, D])
    xe = din("xe", [128, D])
    ctxf = din("ctxf", [CTX, D])
    ctxb = din("ctxb", [CTX, D])
    hv_in = din("hv", [128, 2])
    emask = din("emask", [128, 128])
    invcnt = din("invcnt", [128, 4 * TOK])
    wgs_fm = din("wgs_fm", [128, 9 * 16 * 8])
    bg_s = din("bg_s", [128, 72])
    taps_s = din("taps_s", [128, 11 * 24])
    sel_in = din("sel", [128, 16])
    out = nc.dram_tensor("out", [TOK, D], F32, kind="ExternalOutput").ap()
    modv_out = nc.dram_tensor("modv_out", [128, 48], F32, kind="ExternalOutput").ap() if stop == "hlat_out" else None
    dump_aps = {}
    for nm, shp in dumps:
        dump_aps[nm] = nc.dram_tensor("dbg_" + nm, list(shp), F32, kind="ExternalOutput").ap()

    tk = Tk(nc)
    nc.all_engine_barrier()
    for h in tk.all_sems():
        nc.gpsimd.sem_clear(h)
    nc.all_engine_barrier()
    NW = 53200
    import contextlib
    es = contextlib.ExitStack()
    arena_t = es.enter_context(nc.sbuf_tensor("arena", [128, NW], F32))
    ps_t = es.enter_context(nc.psum_tensor("ps", [128, 4096], F32))
    ar = Arena(arena_t, NW)
    dq = {"n": 0}

    def act(out_, in_, func, reads, writes, bias=None, scale=None, accum=None):
        kw = {}
        if bias is not None:
            kw["bias"] = bias
        if scale is not None:
            kw["scale"] = scale
        if accum is not None:
            kw["accum_out"] = accum
        return tk.op("act", lambda e: e.activation(out=out_, in_=in_, func=func, **kw), reads, writes)

    def ts(eng, out_, in0, s1, s2, op0, op1, reads, writes):
        if op1 is None:
            return tk.op(eng, lambda e: e.tensor_scalar(out=out_, in0=in0, scalar1=s1, scalar2=None, op0=op0), reads, writes)
        return tk.op(eng, lambda e: e.tensor_scalar(out=out_, in0=in0, scalar1=s1, scalar2=s2, op0=op0, op1=op1), reads, writes)

    def tt(eng, out_, in0, in1, op, reads, writes):
        return tk.op(eng, lambda e: e.tensor_tensor(out=out_, in0=in0, in1=in1, op=op), reads, writes)

    def stt(out_, in0, scalar, in1, op0, op1, reads, writes):
        return tk.op("dve", lambda e: e.scalar_tensor_tensor(out=out_, in0=in0, scalar=scalar, in1=in1, op0=op0, op1=op1), reads, writes)

    def cp(eng, out_, in_, reads, writes):
        if eng == "act":
            return act(out_, in_, AF.Copy, reads, writes)
        return tk.op(eng, lambda e: e.tensor_copy(out=out_, in_=in_), reads, writes)

    def mm(out_, lhsT, rhs, start, stop, reads, writes, sig=None):
        if sig is None:
            sig = stop
        return tk.op("pe", lambda e: e.matmul(out_, lhsT=lhsT, rhs=rhs, start=start, stop=stop), reads, writes, sig=sig)

    def trp(out_, in_, ident, reads, writes, sig=True):
        return tk.op("pe", lambda e: e.transpose(out=out_, in_=in_, identity=ident), reads, writes, sig=sig)

    dsems = {}

    def dma(q, out_, in_, reads, writes, sem):
        if sem not in dsems:
            dsems[sem] = tk.dsem(sem)
        return tk.op(q, lambda e: e.dma_start(out=out_, in_=in_), reads, writes, dsem=sem)

    def memset(eng, ap, val, writes):
        return tk.op(eng, lambda e: e.memset(ap, val), (), writes)

    def dump(nm, ap, buf):
        if nm in dump_aps:
            dma("pool", dump_aps[nm], ap, [buf], [], "dump")

    def bank(i):
        return ps_t[:, i * 512:(i + 1) * 512]

    PB = [Buf("pb%d" % i) for i in range(8)]
    P7a = ps_t[:, 7 * 512:7 * 512 + 128].bitcast(BF16)
    P7 = [ps_t[:, 7 * 512 + 256 + 32 * j:7 * 512 + 256 + 32 * (j + 1)] for j in range(8)]
    P7B = [Buf("p7_%d" % j) for j in range(8)]

    ident_bf = ar.alloc(128, BF16)
    ident_f = ar.alloc(128, F32)
    Umask = ar.alloc(128, F32)
    Lmask = ar.alloc(128, F32)
    ones_f = ar.alloc(128, F32)
    ones_bf = ar.alloc(128, BF16)
    CB = Buf("consts")
    memset("pool", ones_f, 1.0, [CB])
    memset("pool", ones_bf, 1.0, [CB])
    tk.op("pool", lambda e: e.affine_select(out=ident_f, in_=ones_f, pattern=[[-1, 128]], compare_op=ALU.is_equal, fill=0.0, base=0, channel_multiplier=1), [CB], [CB])
    tk.op("pool", lambda e: e.affine_select(out=ident_bf, in_=ones_bf, pattern=[[-1, 128]], compare_op=ALU.is_equal, fill=0.0, base=0, channel_multiplier=1), [CB], [CB])
    tk.op("pool", lambda e: e.affine_select(out=Umask, in_=ones_f, pattern=[[1, 128]], compare_op=ALU.is_ge, fill=0.0, base=0, channel_multiplier=-1), [CB], [CB])
    tk.op("pool", lambda e: e.affine_select(out=Lmask, in_=ones_f, pattern=[[-1, 128]], compare_op=ALU.is_ge, fill=0.0, base=0, channel_multiplier=1), [CB], [CB])

    U_bf = ar.alloc(128, BF16)
    L_bf = ar.alloc(128, BF16)
    cp("pool", U_bf, Umask, [CB], [CB])
    cp("pool", L_bf, Lmask, [CB], [CB])

    def load_const(dram, n, q="sp"):
        t = ar.alloc(n, F32)
        dma(q, t, dram, [], [CB], "consts")
        return t

    c_sb = load_const(c_fm, 32)
    bmod_sb = load_const(bmod_fm, 192)
    n1g_sb = load_const(n1g_fm, 32)
    n2g_sb = load_const(n2g_fm, 16)
    wcq_sb = load_const(wcq_fm, 24)
    gbias_sb = load_const(gbias_bc, 16)
    hng_sb = load_const(hng_fm, 8)
    pscale_sb = load_const(pscale_fm, 8)
    bgr_sb = load_const(bgr_bc, 36)
    emask_sb = load_const(emask, 128)
    bgs_sb = load_const(bg_s, 72)
    taps_sb = load_const(taps_s, 264)
    sel_sb = load_const(sel_in, 16)
    wgs_bf = ar.alloc(9 * 16 * 8, BF16, shape=(9, 16))
    dma("pool", wgs_bf, wgs_fm.rearrange("p (s k n) -> p s k n", s=9, k=16), [], [CB], "consts")
    wgr_bf = ar.alloc(16 * 36, BF16, shape=(16,))
    dma("pool", wgr_bf, wgr_fm.rearrange("p (k n) -> p k n", k=16), [], [CB], "consts")

    s_bf = ar.alloc(32, BF16, shape=(16,))
    act(s_bf, c_sb.rearrange("p (k n) -> p k n", k=16), AF.Silu, [CB], [CB])
    mod_sb = ar.alloc(192, F32, shape=(96,))
    ar.group("wmod")
    NB = 16
    CBW = 768
    wm = [ar.alloc(16 * CBW, BF16, shape=(16,)) for _ in range(2)]
    wmb = [Buf("wm0"), Buf("wm1")]
    pmod = bank(0)[:, 0:192]
    for cb in range(NB):
        b = cb % 2
        dma("pool", wm[b], w_mod[:, cb * CBW:(cb + 1) * CBW].rearrange("(k p) n -> p k n", p=128), [], [wmb[b]], "wm%d" % b)
        for j in range(CBW // 128):
            jj = cb * (CBW // 128) + j
            for kc in range(NKC):
                mm(pmod[:, 2 * jj:2 * jj + 2], wm[b][:, kc, j * 128:(j + 1) * 128], s_bf[:, kc, :], kc == 0, kc == NKC - 1,
                   [wmb[b], CB], [PB[0]])
    tt("dve", mod_sb, pmod.rearrange("p (j n) -> p j n", j=96), bmod_sb.rearrange("p (j n) -> p j n", j=96), ALU.add, [PB[0], CB], [CB])
    ar.free_group("wmod")
    ar.group("base")
    tk.barrier()
    gm1 = ar.alloc(32, F32, shape=(16,))
    stt(gm1, mod_sb[:, 16:32, :], 1.0, n1g_sb.rearrange("p (k n) -> p k n", k=16), ALU.add, ALU.mult, [CB], [CB])
    gm2 = ar.alloc(16, F32)
    stt(gm2, mod_sb[:, 64:80, 0], 1.0, n2g_sb, ALU.add, ALU.mult, [CB], [CB])
    sh2 = ar.alloc(16, F32)
    cp("dve", sh2, mod_sb[:, 48:64, 0], [CB], [CB])
    g1f = ar.alloc(16, F32)
    cp("dve", g1f, mod_sb[:, 32:48, 0], [CB], [CB])
    g2f = ar.alloc(16, F32)
    cp("dve", g2f, mod_sb[:, 80:96, 0], [CB], [CB])
    dump("mod", mod_sb.rearrange("p j n -> p (j n)"), CB)
    if stop == "mod":
        return finish(nc, tk, es, dsems)

    ar.group("xp")
    xp_ss = [ar.alloc(32, F32) for _ in range(2)]
    eps_c = ar.alloc(8, F32)
    memset("pool", eps_c, EPS, [CB])
    xp_xb = [ar.alloc(D, BF16) for _ in range(2)]
    xp_b = [Buf("xp0"), Buf("xp1")]
    xpc = {"n": 0}
    P7aB = [Buf("p7a0"), Buf("p7a1")]
    P7a_v = [ps_t[:, 7 * 512 + 128 * j:7 * 512 + 128 * (j + 1)].bitcast(BF16) for j in range(2)]

    def xpath(src, srcb, dst, dstb, gmc, shc):
        i = xpc["n"] % 2
        xpc["n"] += 1
        ss, xb, xb_b = xp_ss[i], xp_xb[i], xp_b[i]
        for q4 in range(4):
            tk.op("dve", lambda e, q4=q4: e.bn_stats(out=ss[:, 8 + q4 * 6:8 + (q4 + 1) * 6], in_=src[:, q4 * 512:(q4 + 1) * 512]), [srcb], [xb_b])
        tk.op("dve", lambda e: e.bn_aggr(out=ss[:, 0:2], in_=ss[:, 8:32]), [xb_b], [xb_b])
        stt(ss[:, 2:3], ss[:, 0:1], ss[:, 0:1], ss[:, 1:2], ALU.mult, ALU.add, [xb_b], [xb_b])
        act(ss[:, 4:5], ss[:, 2:3], AF.Sqrt, [xb_b], [xb_b], bias=eps_c[:, 0:1])
        tk.op("dve", lambda e: e.reciprocal(out=ss[:, 3:4], in_=ss[:, 4:5]), [xb_b], [xb_b])
        act(xb, src, AF.Copy, [srcb, xb_b], [xb_b], scale=ss[:, 3:4])
        for g in range(8):
            pj = g % 2
            for k2 in range(2):
                kc = g * 2 + k2
                trp(P7a_v[pj][:, k2 * 128:(k2 + 1) * 128], xb[:, kc * 128:(kc + 1) * 128], ident_bf, [xb_b, CB], [P7aB[pj]], sig=(k2 == 1))
            for k2 in range(2):
                kc = g * 2 + k2
                if kc % 2 == 0:
                    ts("dve", dst(kc), P7a_v[pj][:, k2 * 128:(k2 + 1) * 128], gmc(kc), shc(kc), ALU.mult, ALU.add, [P7aB[pj], CB], [dstb])
                else:
                    act(dst(kc), P7a_v[pj][:, k2 * 128:(k2 + 1) * 128], AF.Identity, [P7aB[pj], CB], [dstb], bias=shc(kc), scale=gmc(kc))

    gm_lat = lambda kc: gm1[:, kc, 0:1]
    sh_lat = lambda kc: mod_sb[:, kc, 0:1]
    gm_ctx = lambda kc: gm1[:, kc, 1:2]
    sh_ctx = lambda kc: mod_sb[:, kc, 1:2]

    NXS = 2
    xs = [ar.alloc(D, F32) for _ in range(NXS)]
    xsb = [Buf("xs%d" % i) for i in range(NXS)]
    ar.group("base")
    xsc = {"n": 0}

    def load_x(dram_rows):
        i = xsc["n"] % NXS
        xsc["n"] += 1
        dma("sp", xs[i], dram_rows, [], [xsb[i]], "xs%d" % i)
        return xs[i], xsb[i]

    if stop == "xpath":
        xnT = ar.alloc(16 * 128, BF16, shape=(16,))
        xb_ = Buf("xnT")
        src, srcb = load_x(xown[0:128, :])
        xpath(src, srcb, lambda kc: xnT[:, kc, :], xb_, gm_lat, sh_lat)
        o32 = ar.alloc(2048, F32)
        cp("dve", o32, xnT.rearrange("p k t -> p (k t)"), [xb_], [xb_])
        dump("xnT", o32, xb_)
        dump("ss", xp_ss[0], xp_b[0])
        o33 = ar.alloc(2048, F32)
        cp("dve", o33, xp_xb[0], [xp_b[0]], [xp_b[0]])
        dump("xb", o33, xp_b[0])
        return finish(nc, tk, es, dsems)

    def flat(ap):
        n = len(ap.shape)
        if n == 3:
            return ap.rearrange("p a b -> p (a b)")
        if n == 4:
            return ap.rearrange("p a b c -> p (a b c)")
        return ap

    ar.group("states")
    Fst = ar.alloc(2048, F32, shape=(4, 2)); Fn = ar.alloc(8, F32); FB = Buf("F")
    S = ar.alloc(2048, F32, shape=(4, 2)); Sn = ar.alloc(8, F32); SB = Buf("S")
    for t_ in (Fst, S):
        memset("pool", flat(t_), 0.0, [SB])
    for t_ in (Fn, Sn):
        memset("pool", t_, 0.0, [SB])
    FB.w = SB.w
    ar.group("ph1")
    Sc = ar.alloc(2048, F32, shape=(4, 2)); Scn = ar.alloc(8, F32); ScB = Buf("Sc")
    memset("pool", flat(Sc), 0.0, [ScB]); memset("pool", Scn, 0.0, [ScB])
    tmpn = ar.alloc(8, F32)
    Wkv = ar.alloc(16 * 2048, BF16, shape=(16,))
    WkvB = Buf("Wkv")
    for cb in range(4):
        dma("pool", Wkv[:, :, cb * 512:(cb + 1) * 512], w_in[:, K0 + cb * 512:K0 + (cb + 1) * 512].rearrange("(k p) n -> p k n", p=128), [], [WkvB], "wkv")
    xnT_o = ar.alloc(16 * 1024, BF16, shape=(16,))
    xnB = [Buf("xno%d" % i) for i in range(8)]
    gates_sb = ar.alloc(64, F32, shape=(8,))
    GB = Buf("gates")
    uk = [ar.alloc(1040, F32) for _ in range(2)]; ukB = [Buf("uk0"), Buf("uk1")]
    cvbuf = ar.alloc(2048, F32)
    cv = [cvbuf[:, 0:1024], cvbuf[:, 1024:2048]]; cvB = [Buf("cv0"), Buf("cv1")]
    tmpS = cvbuf
    kT = ar.alloc(8 * 1024, BF16, shape=(8,)); kTB = [Buf("kT%d" % m) for m in range(8)]
    Ktm = [ar.alloc(1024, BF16) for _ in range(2)]; KtmB = [Buf("ktm0"), Buf("ktm1")]
    VW = [ar.alloc(1024, BF16, shape=(4,)) for _ in range(2)]
    VWB = [[Buf("vw%d_%d" % (a, h)) for h in range(4)] for a in range(2)]
    GS = []
    for _ in range(2):
        GS.append(dict(e1=ar.alloc(32, F32), nlf=ar.alloc(32, F32), nG=ar.alloc(32, F32), t1=ar.alloc(32, F32),
                       egs=ar.alloc(32, F32), eg=ar.alloc(32, F32), egs_bf=ar.alloc(64, BF16, shape=(32,)),
                       nlf_hi=ar.alloc(32, BF16), nlf_lo=ar.alloc(32, BF16), B=Buf("gmath")))
    kedge = ar.alloc(8 * 128, F32, shape=(8,)); KEB = Buf("kedge")
    xnT_e = ar.alloc(16 * 128, BF16, shape=(16,)); xeB = Buf("xnTe")

    def v3(ap, a):
        return ap.rearrange("p (a b) -> p a b", a=a)

    def gate_math(gs, gi3, gf3, nt, Mmat, ps_b=3, ps_g=4):
        n4 = nt * 4
        e1, nlf, nG_sb, t1, egs, eg, egs_bf, GMB = gs["e1"], gs["nlf"], gs["nG"], gs["t1"], gs["egs"], gs["eg"], gs["egs_bf"], gs["B"]
        act(v3(e1[:, 0:n4], nt), gf3, AF.Exp, [GB], [GMB], scale=-1.0)
        act(nlf[:, 0:n4], e1[:, 0:n4], AF.Ln, [GMB], [GMB], bias=ones_f[:, 0:1])
        nh, nl = gs["nlf_hi"], gs["nlf_lo"]
        cp("dve", nh[:, 0:n4], nlf[:, 0:n4], [GMB], [GMB])
        tt("dve", e1[:, 0:n4], nlf[:, 0:n4], nh[:, 0:n4], ALU.subtract, [GMB], [GMB])
        cp("dve", nl[:, 0:n4], e1[:, 0:n4], [GMB], [GMB])
        Mb = U_bf if Mmat is Umask else L_bf
        mm(P7[ps_b][:, 0:n4], Mb, nh[:, 0:n4], True, False, [GMB, CB], [P7B[ps_b]], sig=False)
        mm(P7[ps_b][:, 0:n4], Mb, nl[:, 0:n4], False, True, [GMB, CB], [P7B[ps_b]])
        mm(P7[ps_g][:, 0:n4], ones_bf, nh[:, 0:n4], True, False, [GMB, CB], [P7B[ps_g]], sig=False)
        mm(P7[ps_g][:, 0:n4], ones_bf, nl[:, 0:n4], False, True, [GMB, CB], [P7B[ps_g]])
        cp("dve", nG_sb[:, 0:n4], P7[ps_g][:, 0:n4], [P7B[ps_g]], [GMB])
        tt("dve", t1[:, 0:n4], P7[ps_b][:, 0:n4], nG_sb[:, 0:n4], ALU.subtract, [P7B[ps_b], GMB], [GMB])
        tt("dve", v3(t1[:, 0:n4], nt), v3(t1[:, 0:n4], nt), gi3, ALU.add, [GMB, GB], [GMB])
        act(egs[:, 0:n4], t1[:, 0:n4], AF.Exp, [GMB], [GMB])
        act(eg[:, 0:n4], nG_sb[:, 0:n4], AF.Exp, [GMB], [GMB], scale=-1.0)
        cp("dve", egs_bf[:, 0:n4, 0], egs[:, 0:n4], [GMB], [GMB])
        cp("dve", egs_bf[:, 0:n4, 1], egs[:, 0:n4], [GMB], [GMB])

    def state_update(gs, col0, Ktm_ap, KtmBuf, vsrc, St, Stn, StB, kb):
        egs, eg, egs_bf, GMB = gs["egs"], gs["eg"], gs["egs_bf"], gs["B"]
        for h in range(4):
            col = col0 + h
            vap, vbuf = vsrc(h)
            ts("dve", VW[kb][:, h, :], vap, egs[:, col:col + 1], None, ALU.mult, None, [vbuf, GMB], [VWB[kb][h]])
        for h in range(4):
            col = col0 + h
            pb = 5 + h % 2
            for half in range(2):
                ksl = Ktm_ap[:, h * 256 + half * 128:h * 256 + (half + 1) * 128]
                mm(bank(pb)[:, half * 256:(half + 1) * 256], ksl, VW[kb][:, h, :], True, True, [KtmBuf, VWB[kb][h]], [PB[pb]], sig=(half == 1))
            for half in range(2):
                ksl = Ktm_ap[:, h * 256 + half * 128:h * 256 + (half + 1) * 128]
                mm(P7[5][:, (h * 2 + half) * 2:(h * 2 + half) * 2 + 2], ksl, egs_bf[:, col, :], True, True, [KtmBuf, GMB], [P7B[5]], sig=(half == 1))
            stt(flat(St[:, h]), flat(St[:, h]), eg[:, col:col + 1], bank(pb), ALU.mult, ALU.add, [StB, GMB, PB[pb]], [StB])
            stt(Stn[:, h * 2:h * 2 + 2], Stn[:, h * 2:h * 2 + 2], eg[:, col:col + 1], v3(P7[5][:, h * 4:h * 4 + 4], 2)[:, :, 0], ALU.mult, ALU.add, [StB, GMB, P7B[5]], [StB])

    def kconv(m, T, slot, W, WBuf, wcol0, xT, xBufs_of, edge_l, edge_r, dst, dstB, func, post_scale=None):
        ub = m % 2
        hw = min(T, 512)
        for h in range(T // hw):
            pb = (m * (T // hw) + h) % 2
            for kc in range(NKC):
                mm(bank(pb)[:, 0:hw], W[:, kc, wcol0 + m * 128:wcol0 + (m + 1) * 128], xT[:, kc, h * hw:(h + 1) * hw], kc == 0, kc == NKC - 1,
                   [WBuf] + xBufs_of(h * hw, (h + 1) * hw), [PB[pb]])
            cp("act", uk[ub][:, 1 + h * hw:1 + (h + 1) * hw], bank(pb)[:, 0:hw], [PB[pb]], [ukB[ub]])
        if edge_l is None:
            memset("pool", uk[ub][:, 0:1], 0.0, [ukB[ub]])
            memset("pool", uk[ub][:, T + 1:T + 2], 0.0, [ukB[ub]])
        else:
            cp("pool", uk[ub][:, 0:1], edge_l[0], [edge_l[1]], [ukB[ub]])
            cp("pool", uk[ub][:, T + 1:T + 2], edge_r[0], [edge_r[1]], [ukB[ub]])
        tap = lambda j: taps_sb[:, slot * 24 + m * 3 + j:slot * 24 + m * 3 + j + 1]
        ts("dve", cv[ub][:, 0:T], uk[ub][:, 1:T + 1], tap(1), None, ALU.mult, None, [ukB[ub], CB], [cvB[ub]])
        stt(cv[ub][:, 0:T], uk[ub][:, 0:T], tap(0), cv[ub][:, 0:T], ALU.mult, ALU.add, [ukB[ub], CB, cvB[ub]], [cvB[ub]])
        stt(cv[ub][:, 0:T], uk[ub][:, 2:T + 2], tap(2), cv[ub][:, 0:T], ALU.mult, ALU.add, [ukB[ub], CB, cvB[ub]], [cvB[ub]])
        if post_scale is None:
            act(dst, cv[ub][:, 0:T], func, [cvB[ub]], [dstB])
        else:
            act(cv[ub][:, 0:T], cv[ub][:, 0:T], func, [cvB[ub]], [cvB[ub]])
            ts("pool", dst, cv[ub][:, 0:T], post_scale, None, ALU.mult, None, [cvB[ub]], [dstB])

    def scan_slot(src_dram, nt, slot, gmc, shc, edge_j, St, Stn, StB):
        T = nt * 128
        for i in range(nt):
            src, srcb = load_x(src_dram[i * 128:(i + 1) * 128, :])
            xpath(src, srcb, lambda kc, i=i: xnT_o[:, kc, i * 128:(i + 1) * 128], xnB[i], gmc, shc)
        for m in range(8):
            if edge_j is None:
                el = er = None
            else:
                el = (kedge[:, m, 2 * edge_j:2 * edge_j + 1], KEB)
                er = (kedge[:, m, 2 * edge_j + 1:2 * edge_j + 2], KEB)
            kconv(m, T, slot, Wkv, WkvB, 0, xnT_o, lambda a, b_: xnB[a // 128:b_ // 128], el, er, kT[:, m, 0:T], kTB[m], AF.Silu)
        if stop == "s_k":
            dump("kT0", kT[:, 0, 0:T], kTB[0])
            return
        for i in range(nt):
            kb = i % 2
            gs = GS[kb]
            for kc in range(NKC):
                mm(P7[2][:, 0:8], xnT_o[:, kc, i * 128:(i + 1) * 128], wgs_bf[:, slot, kc, :], kc == 0, kc == NKC - 1, [xnB[i], CB], [P7B[2]])
            tt("dve", gates_sb[:, kb, :], P7[2][:, 0:8], bgs_sb[:, slot * 8:(slot + 1) * 8], ALU.add, [P7B[2], CB], [GB])
            gate_math(gs, gates_sb[:, kb:kb + 1, 0:4], gates_sb[:, kb:kb + 1, 4:8], 1, Umask)
            if stop == "s_g":
                dump("egs", gs["egs"], gs["B"]); dump("eg", gs["eg"], gs["B"]); dump("gates", flat(gates_sb), GB)
                return
            for ch in range(2):
                pb = 2 + ch
                for kc in range(NKC):
                    mm(bank(pb), xnT_o[:, kc, i * 128:(i + 1) * 128], Wkv[:, kc, 1024 + ch * 512:1024 + (ch + 1) * 512], kc == 0, kc == NKC - 1,
                       [xnB[i], WkvB], [PB[pb]])
            b4 = bank(4).bitcast(BF16)
            for m in range(8):
                trp(b4[:, m * 128:(m + 1) * 128], kT[:, m, i * 128:(i + 1) * 128], ident_bf, [kTB[m], CB], [PB[4]], sig=(m == 7))
            cp("act", Ktm[kb], b4, [PB[4]], [KtmB[kb]])
            if stop == "s_t":
                return
            if stop == "s_u" and i == 1:
                return
            state_update(gs, 0, Ktm[kb], KtmB[kb], lambda h: (bank(2 + h // 2)[:, (h % 2) * 256:(h % 2 + 1) * 256], PB[2 + h // 2]),
                         St, Stn, StB, kb)

    def boundary(j):
        selc = sel_sb[:, 2 * j:2 * j + 1]
        nselc = sel_sb[:, 2 * j + 1:2 * j + 2]
        stt(flat(Fst), flat(S), selc, flat(Fst), ALU.mult, ALU.add, [SB, FB, CB], [FB])
        stt(Fn, Sn, selc, Fn, ALU.mult, ALU.add, [SB, FB, CB], [FB])
        ts("dve", tmpS, flat(Sc), selc, None, ALU.mult, None, [ScB, CB], cvB)
        ts("dve", tmpn, Scn, selc, None, ALU.mult, None, [ScB, CB], cvB)
        stt(flat(S), flat(S), nselc, tmpS, ALU.mult, ALU.add, [SB, CB] + cvB, [SB])
        stt(Sn, Sn, nselc, tmpn, ALU.mult, ALU.add, [SB, CB] + cvB, [SB])

    src, srcb = load_x(xe[:, :])
    xpath(src, srcb, lambda kc: xnT_e[:, kc, :], xeB, gm_lat, sh_lat)
    for m in range(8):
        pb = m % 2
        for kc in range(NKC):
            mm(bank(pb)[:, 0:128], Wkv[:, kc, m * 128:(m + 1) * 128], xnT_e[:, kc, :], kc == 0, kc == NKC - 1, [WkvB, xeB], [PB[pb]])
        tt("dve", kedge[:, m, :], bank(pb)[:, 0:128], emask_sb, ALU.mult, [PB[pb], CB], [KEB])

    if stop == "edge":
        dump("kedge", flat(kedge), KEB)
        return finish(nc, tk, es, dsems)
    scan_slot(ctxf, 2, 0, gm_ctx, sh_ctx, None, S, Sn, SB)
    if stop in ("s_k", "s_g", "s_v", "s_t", "s_u"):
        dump("S", flat(S), SB); dump("Sn", Sn, SB)
        return finish(nc, tk, es, dsems)
    scan_slot(ctxb, 2, 1, gm_ctx, sh_ctx, None, Sc, Scn, ScB)
    if stop == "ctx":
        dump("S", flat(S), SB); dump("Sn", Sn, SB); dump("Sc", flat(Sc), ScB); dump("Scn", Scn, ScB)
        return finish(nc, tk, es, dsems)
    NSLOT = 7 if stop != "slot1" else 1
    for j in range(NSLOT):
        boundary(j)
        scan_slot(xo[j * TOK:(j + 1) * TOK, :], 8, 2 + j, gm_lat, sh_lat, j, S, Sn, SB)
    boundary(7)
    if stop in ("others", "slot1"):
        dump("F", flat(Fst), FB); dump("Fn", Fn, FB); dump("B", flat(S), SB); dump("Bn", Sn, SB)
        return finish(nc, tk, es, dsems)
    tk.barrier()
    ar.free_group("ph1")

    ar.group("own_y")
    yT = ar.alloc(8 * 1024, BF16, shape=(8,)); yTB = [Buf("yT%d" % m) for m in range(8)]
    hv = ar.alloc(2, F32)
    hedge = ar.alloc(32, BF16, shape=(16,)); hedgeB = Buf("hedge")
    dma("sp", hv, hv_in, [], [CB], "consts")
    ar.group("proj")
    xnT = ar.alloc(16 * 1024, BF16, shape=(16,)); xnTB = [Buf("xnT%d" % i) for i in range(8)]
    Wb = [ar.alloc(16 * 512, BF16, shape=(16,)) for _ in range(2)]; WbB = [Buf("Wb0"), Buf("Wb1")]
    wbn = {"n": 0}

    def load_w(dram, col0, ncols=512):
        i = wbn["n"] % 2
        wbn["n"] += 1
        dma("pool", Wb[i][:, :, 0:ncols], dram[:, col0:col0 + ncols].rearrange("(k p) n -> p k n", p=128), [], [WbB[i]], "wb%d" % i)
        return Wb[i], WbB[i]

    for i in range(NT):
        src, srcb = load_x(xown[i * 128:(i + 1) * 128, :])
        xpath(src, srcb, lambda kc, i=i: xnT[:, kc, i * 128:(i + 1) * 128], xnTB[i], gm_lat, sh_lat)
    xnT_of = lambda a, b_: xnTB[a // 128:b_ // 128]

    ar.group("poolst")
    uP = ar.alloc(8 * 2048, BF16, shape=(8,)); uPB = [Buf("uP%d" % m) for m in range(8)]
    xh = [ar.alloc(16 * 128, BF16, shape=(16,)) for _ in range(2)]; xhB = [Buf("xh0"), Buf("xh1")]
    inv_g = [ar.alloc(1024, F32) for _ in range(2)]; invB = [Buf("inv0"), Buf("inv1")]
    box = [ar.alloc(31 * 80, F32) for _ in range(2)]; boxB = Buf("box")
    wp_bf = ar.alloc(4 * 2 * 256, BF16, shape=(4, 2)); wpB = Buf("wp")
    dma("pool", wp_bf, w_pool.rearrange("g (cc p) d -> p g cc d", p=128), [], [wpB], "wp")
    Wp = [load_w(w_in, 0), load_w(w_in, 512)]
    for t in range(8):
        src, srcb = load_x(xhalo[t * 128:(t + 1) * 128, :])
        xb_ = t % 2
        xpath(src, srcb, lambda kc, xb_=xb_: xh[xb_][:, kc, :], xhB[xb_], gm_lat, sh_lat)
        e0 = t * 128 if t < 4 else 1536 + (t - 4) * 128
        hvc = hv[:, 0:1] if t < 4 else hv[:, 1:2]
        for m in range(8):
            W_, WB_ = Wp[m // 4]
            mc = m % 4
            pb = m % 2
            for kc in range(NKC):
                mm(bank(pb)[:, 0:128], W_[:, kc, mc * 128:(mc + 1) * 128], xh[xb_][:, kc, :], kc == 0, kc == NKC - 1, [WB_, xhB[xb_]], [PB[pb]])
            if m % 2 == 0:
                ts("dve", uP[:, m, e0:e0 + 128], bank(pb)[:, 0:128], hvc, None, ALU.mult, None, [PB[pb], CB], [uPB[m]])
            else:
                act(uP[:, m, e0:e0 + 128], bank(pb)[:, 0:128], AF.Copy, [PB[pb], CB], [uPB[m]], scale=hvc)
        if t == 3:
            cp("pool", hedge[:, :, 0], xh[xb_][:, :, 127], [xhB[xb_]], [hedgeB])
        if t == 4:
            cp("pool", hedge[:, :, 1], xh[xb_][:, :, 0], [xhB[xb_]], [hedgeB])
    for m in range(8):
        W_, WB_ = Wp[m // 4]
        mc = m % 4
        for h in range(2):
            pb = h
            for kc in range(NKC):
                mm(bank(pb), W_[:, kc, mc * 128:(mc + 1) * 128], xnT[:, kc, h * 512:(h + 1) * 512], kc == 0, kc == NKC - 1,
                   [WB_] + xnTB[4 * h:4 * h + 4], [PB[pb]])
            cp("act" if h == 0 else "dve", uP[:, m, 512 + h * 512:512 + (h + 1) * 512], bank(pb), [PB[pb]], [uPB[m]])
    for m in range(8):
        g = m // 2
        w = WINS[g]
        lo = w // 2
        R0 = 8 - lo
        nr = 16 + w - 1
        L = nr * 80
        if m % 2 == 0:
            dma("sp", inv_g[g % 2], invcnt[:, g * 1024:(g + 1) * 1024], [], [invB[g % 2]], "inv%d" % (g % 2))
        cur, nxt = box[0], box[1]
        memset("pool", cur[:, 0:L], 0.0, [boxB])
        c3 = cur[:, 0:L].rearrange("p (r c) -> p r c", c=80)
        cp("act", c3[:, :, 8:72], uP[:, m, R0 * 64:(R0 + nr) * 64].rearrange("p (r c) -> p r c", c=64), [uPB[m]], [boxB])
        s = 1
        while s < w:
            tt("dve", nxt[:, 0:L - s], cur[:, 0:L - s], cur[:, s:L], ALU.add, [boxB], [boxB])
            memset("pool", nxt[:, L - s:L], 0.0, [boxB])
            cur, nxt = nxt, cur
            s *= 2
        s = 1
        while s < w:
            tt("dve", nxt[:, 0:L - s * 80], cur[:, 0:L - s * 80], cur[:, s * 80:L], ALU.add, [boxB], [boxB])
            cur, nxt = nxt, cur
            s *= 2
        c3 = cur[:, 0:L].rearrange("p (r c) -> p r c", c=80)
        vs = c3[:, 0:16, 8 - lo:8 - lo + 64]
        n3 = nxt[:, 0:1024].rearrange("p (r c) -> p r c", c=64)
        tt("dve", n3, vs, inv_g[g % 2].rearrange("p (r c) -> p r c", c=64), ALU.mult, [boxB, invB[g % 2]], [boxB])
        tt("dve", uP[:, m, 512:1536], nxt[:, 0:1024], uP[:, m, 512:1536], ALU.subtract, [boxB, uPB[m]], [uPB[m]])
    if stop == "poold":
        dump("dT", uP[:, 0, 512:1536], uPB[0]); dump("dT7", uP[:, 7, 512:1536], uPB[7])
        return finish(nc, tk, es, dsems)
    for g in range(4):
        for dc in range(2):
            for h in range(2):
                pb = (dc * 2 + h) % 2
                for cc in range(2):
                    mm(bank(pb), wp_bf[:, g, cc, dc * 128:(dc + 1) * 128], uP[:, 2 * g + cc, 512 + h * 512:512 + (h + 1) * 512], cc == 0, cc == 1,
                       [wpB, uPB[2 * g + cc]], [PB[pb]])
                mo = 2 * g + dc
                if h == 0:
                    ts("dve", yT[:, mo, h * 512:(h + 1) * 512], bank(pb), pscale_sb[:, mo:mo + 1], None, ALU.mult, None, [PB[pb], CB], [yTB[mo]])
                else:
                    act(yT[:, mo, h * 512:(h + 1) * 512], bank(pb), AF.Copy, [PB[pb], CB], [yTB[mo]], scale=pscale_sb[:, mo:mo + 1])
    if stop == "pool":
        dump("yT0", yT[:, 0, :], yTB[0]); dump("yT7", yT[:, 7, :], yTB[7])
        return finish(nc, tk, es, dsems)
    tk.barrier()
    ar.free_group("poolst")
    ar.free_group("xp")

    ar.group("own_out")
    qT = ar.alloc(8 * 1024, BF16, shape=(8,)); qTB = [Buf("qT%d" % m) for m in range(8)]
    kTo = ar.alloc(8 * 1024, BF16, shape=(8,)); kToB = [Buf("kTo%d" % m) for m in range(8)]
    sigoT = ar.alloc(8 * 1024, BF16, shape=(8,)); sigoB = [Buf("so%d" % m) for m in range(8)]
    Ktm_all = ar.alloc(8 * 1024, BF16, shape=(8,)); KtmAB = [Buf("ktma%d" % i) for i in range(8)]
    V_own = ar.alloc(8 * 1024, BF16, shape=(8,)); VoB = [Buf("vo%d" % i) for i in range(8)]
    gates_own = ar.alloc(128, F32, shape=(8,)); GoB = Buf("gown")
    wg16 = ar.alloc(16 * 16, BF16, shape=(16,)); wg16B = Buf("wg16")
    dma("pool", wg16, w_in[:, G0:G0 + 16].rearrange("(k p) n -> p k n", p=128), [], [wg16B], "wg16")
    qedge = ar.alloc(16, F32, shape=(8,)); qeB = Buf("qedge")
    ar.group("convs")
    uk0_ = ar.alloc(1040, F32); uk = [uk0_, uk0_]; ukb_ = Buf("uk0b"); ukB = [ukb_, ukb_]
    cv0_ = ar.alloc(1024, F32); cv = [cv0_, cv0_]; cvb_ = Buf("cv0b"); cvB = [cvb_, cvb_]

    def edge_proj(W_, WB_, mc, m):
        for kc in range(NKC):
            mm(P7[1][:, 0:2], W_[:, kc, mc * 128:(mc + 1) * 128], hedge[:, kc, :], kc == 0, kc == NKC - 1, [WB_, hedgeB], [P7B[1]])
        tt("dve", qedge[:, m, :], P7[1][:, 0:2], hv[:, 0:2], ALU.mult, [P7B[1], CB], [qeB])

    Wq = [load_w(w_in, Q0), load_w(w_in, Q0 + 512)]
    for m in range(8):
        W_, WB_ = Wq[m // 4]
        edge_proj(W_, WB_, m % 4, m)
        kconv(m, TOK, 10, W_, WB_, -512 * (m // 4), xnT, xnT_of, (qedge[:, m, 0:1], qeB), (qedge[:, m, 1:2], qeB), qT[:, m, :], qTB[m], AF.Silu,
              post_scale=0.0625)
    Wo = [load_w(w_in, O0), load_w(w_in, O0 + 512)]
    for m in range(8):
        W_, WB_ = Wo[m // 4]
        mc = m % 4
        for h in range(2):
            pb = h
            for kc in range(NKC):
                mm(bank(pb), W_[:, kc, mc * 128:(mc + 1) * 128], xnT[:, kc, h * 512:(h + 1) * 512], kc == 0, kc == NKC - 1,
                   [WB_] + xnTB[4 * h:4 * h + 4], [PB[pb]])
            act(sigoT[:, m, h * 512:(h + 1) * 512], bank(pb), AF.Sigmoid, [PB[pb]], [sigoB[m]])
    Wk = [load_w(w_in, K0), load_w(w_in, K0 + 512)]
    for m in range(8):
        W_, WB_ = Wk[m // 4]
        edge_proj(W_, WB_, m % 4, m)
        kconv(m, TOK, 9, W_, WB_, -512 * (m // 4), xnT, xnT_of, (qedge[:, m, 0:1], qeB), (qedge[:, m, 1:2], qeB), kTo[:, m, :], kToB[m], AF.Silu)
    for i in range(NT):
        b4 = bank(4).bitcast(BF16)
        for m in range(8):
            trp(b4[:, m * 128:(m + 1) * 128], kTo[:, m, i * 128:(i + 1) * 128], ident_bf, [kToB[m], CB], [PB[4]], sig=(m == 7))
        cp("act" if i % 2 == 0 else "dve", Ktm_all[:, i, :], b4, [PB[4]], [KtmAB[i]])
    for ch in range(2):
        W_, WB_ = load_w(w_in, V0 + ch * 512)
        for i in range(NT):
            pb = 2 + i % 2
            for kc in range(NKC):
                mm(bank(pb), xnT[:, kc, i * 128:(i + 1) * 128], W_[:, kc, :], kc == 0, kc == NKC - 1, [xnTB[i], WB_], [PB[pb]])
            cp("act" if i % 2 == 0 else "dve", V_own[:, i, ch * 512:(ch + 1) * 512], bank(pb), [PB[pb]], [VoB[i]])
    for i in range(NT):
        for kc in range(NKC):
            mm(P7[2][:, 0:16], xnT[:, kc, i * 128:(i + 1) * 128], wg16[:, kc, :], kc == 0, kc == NKC - 1, [xnTB[i], wg16B], [P7B[2]])
        tt("dve", gates_own[:, i, :], P7[2][:, 0:16], gbias_sb, ALU.add, [P7B[2], CB], [GoB])
    if stop == "proj":
        dump("qT0", qT[:, 0, :], qTB[0]); dump("kT0", kTo[:, 0, :], kToB[0]); dump("so0", sigoT[:, 0, :], sigoB[0])
        dump("V0", V_own[:, 0, :], VoB[0]); dump("gates", flat(gates_own), GoB); dump("Ktm0", Ktm_all[:, 0, :], KtmAB[0])
        return finish(nc, tk, es, dsems)
    tk.barrier()
    ar.free_group("convs")
    ar.free_group("proj")

    ar.group("scan")
    hF = ar.alloc(8 * 1024, F32, shape=(8, 4)); hFB = [Buf("hF%d" % i) for i in range(8)]
    mT = ar.alloc(8 * 1024, BF16, shape=(8,)); mTB = [Buf("mT%d" % m) for m in range(8)]
    Sb = ar.alloc(2048, BF16, shape=(4, 2)); Snb = ar.alloc(16, BF16, shape=(8,)); SbB = Buf("Sb")
    SpT = [ar.alloc(128, BF16) for _ in range(4)]; SpTB = [Buf("SpT%d" % h) for h in range(4)]
    VW = [ar.alloc(1024, BF16, shape=(4,)) for _ in range(2)]
    GS = []
    for _ in range(2):
        GS.append(dict(e1=ar.alloc(32, F32), nlf=ar.alloc(32, F32), nG=ar.alloc(32, F32), t1=ar.alloc(32, F32),
                       egs=ar.alloc(32, F32), eg=ar.alloc(32, F32), egs_bf=ar.alloc(64, BF16, shape=(32,)),
                       nb=ar.alloc(32, F32), es=ar.alloc(32, F32), eb=ar.alloc(32, F32),
                       nlf_hi=ar.alloc(32, BF16), nlf_lo=ar.alloc(32, BF16), B=Buf("gmath2")))
    dn = ar.alloc(8, F32); rr = ar.alloc(8, F32); rB = Buf("r")
    bst = ar.alloc(24, F32); mv = ar.alloc(8, F32); sd4 = ar.alloc(8, F32); rs4 = ar.alloc(8, F32); nb4 = ar.alloc(8, F32); stB = Buf("lnstat")
    hn = ar.alloc(1024, BF16); hnB = Buf("hn")
    GB = GoB
    for dirn in range(2):
        gs = GS[dirn]
        gate_math(gs, gates_own[:, :, dirn * 8:dirn * 8 + 4], gates_own[:, :, dirn * 8 + 4:dirn * 8 + 8], NT, Umask if dirn == 0 else Lmask,
                  ps_b=3 + 2 * dirn, ps_g=4 + 2 * dirn)
        cp("dve", gs["nb"], P7[3 + 2 * dirn], [P7B[3 + 2 * dirn]], [gs["B"]])
        tt("dve", v3(gs["t1"], NT), v3(gs["nb"], NT), gates_own[:, :, dirn * 8:dirn * 8 + 4], ALU.add, [gs["B"], GoB], [gs["B"]])
        act(gs["es"], gs["t1"], AF.Exp, [gs["B"]], [gs["B"]])
        act(gs["eb"], gs["nb"], AF.Exp, [gs["B"]], [gs["B"]], scale=-1.0)
    if stop == "gm":
        dump("es", GS[0]["es"], GS[0]["B"]); dump("eb", GS[0]["eb"], GS[0]["B"]); dump("esb", GS[1]["es"], GS[1]["B"]); dump("ebb", GS[1]["eb"], GS[1]["B"])
        return finish(nc, tk, es, dsems)

    def refresh_state_bf(St, Stn, StB):
        cp("act", flat(Sb), flat(St), [StB], [SbB])
        cp("dve", Snb[:, :, 0], Stn, [StB], [SbB])
        cp("dve", Snb[:, :, 1], Stn, [StB], [SbB])

    def readout(i):
        for h in range(4):
            tk.op("dve", lambda e, h=h: e.bn_stats(out=bst[:, h * 6:(h + 1) * 6], in_=hF[:, i, h, :]), [hFB[i]], [stB])
            tk.op("dve", lambda e, h=h: e.bn_aggr(out=mv[:, h * 2:(h + 1) * 2], in_=bst[:, h * 6:(h + 1) * 6]), [stB], [stB])
        mv3 = v3(mv, 4)
        act(sd4[:, 0:4], mv3[:, :, 1], AF.Sqrt, [stB], [stB], bias=eps_c[:, 0:1])
        tk.op("dve", lambda e: e.reciprocal(out=rs4[:, 0:4], in_=sd4[:, 0:4]), [stB], [stB])
        stt(nb4[:, 0:4], mv3[:, :, 0], -1.0, rs4[:, 0:4], ALU.mult, ALU.mult, [stB], [stB])
        for h in range(4):
            act(hn[:, h * 256:(h + 1) * 256], hF[:, i, h, :], AF.Identity, [hFB[i], stB], [hnB], bias=nb4[:, h:h + 1], scale=rs4[:, h:h + 1])
        b4 = bank(4).bitcast(BF16)
        for m_ in range(8):
            trp(b4[:, m_ * 128:(m_ + 1) * 128], hn[:, m_ * 128:(m_ + 1) * 128], ident_bf, [hnB, CB], [PB[4]], sig=(m_ == 7))
        for m_ in range(8):
            stt(mT[:, m_, i * 128:(i + 1) * 128], b4[:, m_ * 128:(m_ + 1) * 128], hng_sb[:, m_:m_ + 1], sigoT[:, m_, i * 128:(i + 1) * 128],
                ALU.mult, ALU.mult, [PB[4], CB, sigoB[m_]], [mTB[m_]])

    for dirn in range(2):
        gs = GS[dirn]
        St, Stn, StB = (Fst, Fn, FB) if dirn == 0 else (S, Sn, SB)
        Mask = Umask if dirn == 0 else Lmask
        refresh_state_bf(St, Stn, StB)
        order = list(range(NT)) if dirn == 0 else list(range(NT - 1, -1, -1))
        for i in order:
            tsl = slice(i * 128, (i + 1) * 128)
            for h in range(4):
                col = i * 4 + h
                for half in range(2):
                    mm(bank(0)[:, h * 128:(h + 1) * 128], kTo[:, 2 * h + half, tsl], qT[:, 2 * h + half, tsl], half == 0, half == 1,
                       [kToB[2 * h + half], qTB[2 * h + half]], [PB[0]])
                stt(SpT[h], bank(0)[:, h * 128:(h + 1) * 128], gs["es"][:, col:col + 1], Mask, ALU.mult, ALU.mult, [PB[0], gs["B"], CB], [SpTB[h]])
                accb = 1 + h // 2
                acc = bank(accb)[:, (h % 2) * 256:(h % 2 + 1) * 256]
                mm(acc, SpT[h], V_own[:, i, h * 256:(h + 1) * 256], True, False, [SpTB[h], VoB[i]], [PB[accb]], sig=False)
                mm(acc, qT[:, 2 * h, tsl], Sb[:, h, 0, :], False, False, [qTB[2 * h], SbB], [PB[accb]], sig=False)
                mm(acc, qT[:, 2 * h + 1, tsl], Sb[:, h, 1, :], False, True, [qTB[2 * h + 1], SbB], [PB[accb]])
                den = P7[0][:, h * 2:h * 2 + 2]
                mm(den, SpT[h], ones_bf[:, 0:2], True, False, [SpTB[h], CB], [P7B[0]], sig=False)
                mm(den, qT[:, 2 * h, tsl], Snb[:, 2 * h, :], False, False, [qTB[2 * h], SbB], [P7B[0]], sig=False)
                mm(den, qT[:, 2 * h + 1, tsl], Snb[:, 2 * h + 1, :], False, True, [qTB[2 * h + 1], SbB], [P7B[0]])
            tt("dve", dn[:, 0:4], v3(P7[0][:, 0:8], 4)[:, :, 0], gs["eb"][:, i * 4:i * 4 + 4], ALU.mult, [P7B[0], gs["B"]], [rB])
            ts("dve", dn[:, 4:8], dn[:, 0:4], -1.0, None, ALU.mult, None, [rB], [rB])
            tt("dve", dn[:, 0:4], dn[:, 0:4], dn[:, 4:8], ALU.max, [rB], [rB])
            ts("dve", dn[:, 0:4], dn[:, 0:4], 1.0, None, ALU.max, None, [rB], [rB])
            tk.op("dve", lambda e: e.reciprocal(out=rr[:, 4:8], in_=dn[:, 0:4]), [rB], [rB])
            tt("dve", rr[:, 0:4], rr[:, 4:8], gs["eb"][:, i * 4:i * 4 + 4], ALU.mult, [rB, gs["B"]], [rB])
            for h in range(4):
                accb = 1 + h // 2
                acc = bank(accb)[:, (h % 2) * 256:(h % 2 + 1) * 256]
                if dirn == 0:
                    if h % 2 == 0:
                        ts("dve", hF[:, i, h, :], acc, rr[:, h:h + 1], None, ALU.mult, None, [PB[accb], rB], [hFB[i]])
                    else:
                        act(hF[:, i, h, :], acc, AF.Copy, [PB[accb], rB], [hFB[i]], scale=rr[:, h:h + 1])
                else:
                    stt(hF[:, i, h, :], acc, rr[:, h:h + 1], hF[:, i, h, :], ALU.mult, ALU.add, [PB[accb], rB, hFB[i]], [hFB[i]])
            if dirn == 1:
                readout(i)
            kb = i % 2
            state_update(gs, i * 4, Ktm_all[:, i, :], KtmAB[i], lambda h, i=i: (V_own[:, i, h * 256:(h + 1) * 256], VoB[i]), St, Stn, StB, kb)
            refresh_state_bf(St, Stn, StB)
    if stop == "scan":
        dump("mT0", mT[:, 0, :], mTB[0]); dump("mT7", mT[:, 7, :], mTB[7]); dump("hs0", flat(hF[:, 0]), hFB[0])
        return finish(nc, tk, es, dsems)
    tk.barrier()
    ar.free_group("own_out")
    ar.free_group("states")

    ar.group("hlat")
    h_lat = ar.alloc(8 * 2048, F32, shape=(8,)); hlB = [Buf("hl%d" % i) for i in range(8)]
    for i in range(NT):
        dma("sp", h_lat[:, i, :], xown[i * 128:(i + 1) * 128, :], [], [hlB[i]], "hl%d" % i)
    ar.group("wout")
    g1bc = ar.alloc(2048, F32); g1B = Buf("g1bc")
    tmpb = ar.alloc(128, F32); tmpbB = Buf("tmpb"); tmpb_hi = ar.alloc(128, BF16); tmpb_lo = ar.alloc(128, BF16)
    tmpo = [ar.alloc(512, F32) for _ in range(2)]; tmpoB = [Buf("tmpo0"), Buf("tmpo1")]
    Wb = [ar.alloc(16 * 512, BF16, shape=(16,)) for _ in range(2)]; WbB = [Buf("Wc0"), Buf("Wc1")]

    def row_bcast(dst, dstB, vec_fm):
        for kc in range(NKC):
            ts("dve", tmpb, ones_f, vec_fm[:, kc:kc + 1], None, ALU.mult, None, [CB, tmpbB], [tmpbB])
            cp("dve", tmpb_hi, tmpb, [tmpbB], [tmpbB])
            tt("dve", tmpb, tmpb, tmpb_hi, ALU.subtract, [tmpbB], [tmpbB])
            cp("dve", tmpb_lo, tmpb, [tmpbB], [tmpbB])
            pb = kc % 2
            mm(bank(pb)[:, 0:128], tmpb_hi, ident_bf, True, False, [tmpbB, CB], [PB[pb]], sig=False)
            mm(bank(pb)[:, 0:128], tmpb_lo, ident_bf, False, True, [tmpbB, CB], [PB[pb]])
            cp("act", dst[:, kc * 128:(kc + 1) * 128], bank(pb)[:, 0:128], [PB[pb]], [dstB])

    row_bcast(g1bc, g1B, g1f)
    mixc = lambda kc: (yT[:, kc], yTB[kc]) if kc < 8 else (mT[:, kc - 8], mTB[kc - 8])
    for cb in range(4):
        W_, WB_ = load_w(w_out, cb * 512)
        for i in range(NT):
            pb = i % 4
            for kc in range(NKC):
                ma, mb = mixc(kc)
                mm(bank(pb), ma[:, i * 128:(i + 1) * 128], W_[:, kc, :], kc == 0, kc == NKC - 1, [mb, WB_], [PB[pb]])
            tb = i % 2
            tt("dve", tmpo[tb], bank(pb), g1bc[:, cb * 512:(cb + 1) * 512], ALU.mult, [PB[pb], g1B], [tmpoB[tb]])
            tt("pool", h_lat[:, i, cb * 512:(cb + 1) * 512], h_lat[:, i, cb * 512:(cb + 1) * 512], tmpo[tb], ALU.add, [tmpoB[tb], hlB[i]], [hlB[i]])
    if stop == "hlat_out":
        for i in range(NT):
            dma("pool", out[i * 128:(i + 1) * 128, :], h_lat[:, i, :], [hlB[i]], [], "hlo%d" % (i % 2))
        modv_sb = ar.alloc(48, F32)
        cp("dve", modv_sb[:, 0:16], gm2, [CB], [CB]); cp("dve", modv_sb[:, 16:32], sh2, [CB], [CB]); cp("dve", modv_sb[:, 32:48], g2f, [CB], [CB])
        dma("pool", modv_out, modv_sb, [CB], [], "hlo0")
        return finish(nc, tk, es, dsems)
    if stop == "hlat":
        dump("hlat", h_lat[:, 0, :], hlB[0]); dump("hlat7", h_lat[:, 7, :], hlB[7])
        return finish(nc, tk, es, dsems)
    tk.barrier()
    ar.free_group("wout")
    ar.free_group("scan")
    ar.free_group("own_y")

    ar.group("xp")
    xp_ss = [ar.alloc(32, F32) for _ in range(2)]
    eps_c = ar.alloc(8, F32)
    memset("pool", eps_c, EPS, [CB])
    xp_xb = [ar.alloc(D, BF16) for _ in range(2)]
    xp_b = [Buf("xq0"), Buf("xq1")]
    ar.group("moe")
    fnT = ar.alloc(16 * 1024, BF16, shape=(16,)); fnB = [Buf("fn%d" % i) for i in range(8)]
    gm2c = lambda kc: gm2[:, kc:kc + 1]
    sh2c = lambda kc: sh2[:, kc:kc + 1]
    for i in range(NT):
        xpath(h_lat[:, i, :], hlB[i], lambda kc, i=i: fnT[:, kc, i * 128:(i + 1) * 128], fnB[i], gm2c, sh2c)
    lg = ar.alloc(8 * 36, F32, shape=(8,)); RB = Buf("router")
    for i in range(NT):
        for kc in range(NKC):
            mm(bank(0)[:, 0:36], fnT[:, kc, i * 128:(i + 1) * 128], wgr_bf[:, kc, :], kc == 0, kc == NKC - 1, [fnB[i], CB], [PB[0]])
        tt("dve", lg[:, i, :], bank(0)[:, 0:36], bgr_sb, ALU.add, [PB[0], CB], [RB])
    comb = ar.alloc(8 * 32, F32, shape=(8, 4))
    gmx = ar.alloc(8, F32); goh = ar.alloc(32, F32, shape=(8,)); gex = ar.alloc(32, F32, shape=(8,)); gsum = ar.alloc(8, F32); pg = ar.alloc(8, F32)
    selg = ar.alloc(64, F32, shape=(8,)); tmp32 = ar.alloc(256, F32, shape=(8, 4))
    m1 = ar.alloc(8, F32); m2 = ar.alloc(8, F32); k1 = ar.alloc(64, F32, shape=(8,)); k2 = ar.alloc(64, F32, shape=(8,)); sel2 = ar.alloc(64, F32, shape=(8,))
    p1 = ar.alloc(8, F32); p2 = ar.alloc(8, F32); wk = ar.alloc(64, F32, shape=(8,))
    gl = lg[:, :, 0:4]
    el = lg[:, :, 4:36].rearrange("p t (g e) -> p t g e", g=4)
    R = [RB]
    tk.op("dve", lambda e: e.tensor_reduce(out=gmx, in_=gl, axis=AX.X, op=ALU.max), R, R)
    bc = lambda ap, shp: ap.broadcast_to(shp)
    tt("dve", goh, gl, gmx.unsqueeze(2).broadcast_to([128, 8, 4]), ALU.is_equal, R, R)
    tt("dve", gex, gl, gmx.unsqueeze(2).broadcast_to([128, 8, 4]), ALU.subtract, R, R)
    act(gex, gex, AF.Exp, R, R)
    tk.op("dve", lambda e: e.tensor_reduce(out=gsum, in_=gex, axis=AX.X, op=ALU.add), R, R)
    tk.op("dve", lambda e: e.reciprocal(out=pg, in_=gsum), R, R)
    tt("dve", tmp32, el, goh.unsqueeze(3).broadcast_to([128, 8, 4, 8]), ALU.mult, R, R)
    tk.op("dve", lambda e: e.tensor_reduce(out=selg, in_=tmp32.rearrange("p t g e -> p t e g"), axis=AX.X, op=ALU.add), R, R)
    tk.op("dve", lambda e: e.tensor_reduce(out=m1, in_=selg, axis=AX.X, op=ALU.max), R, R)
    tt("dve", k1, selg, m1.unsqueeze(2).broadcast_to([128, 8, 8]), ALU.is_equal, R, R)
    stt(sel2, k1, -1e30, selg, ALU.mult, ALU.add, R, R)
    tk.op("dve", lambda e: e.tensor_reduce(out=m2, in_=sel2, axis=AX.X, op=ALU.max), R, R)
    tt("dve", k2, sel2, m2.unsqueeze(2).broadcast_to([128, 8, 8]), ALU.is_equal, R, R)
    tt("dve", p1, m1, m2, ALU.subtract, R, R)
    act(p1, p1, AF.Sigmoid, R, R)
    ts("dve", p2, p1, -1.0, 1.0, ALU.mult, ALU.add, R, R)
    tt("dve", p1, p1, pg, ALU.mult, R, R)
    tt("dve", p2, p2, pg, ALU.mult, R, R)
    tt("dve", wk, k1, p1.unsqueeze(2).broadcast_to([128, 8, 8]), ALU.mult, R, R)
    tt("dve", k2, k2, p2.unsqueeze(2).broadcast_to([128, 8, 8]), ALU.mult, R, R)
    tt("dve", wk, wk, k2, ALU.add, R, R)
    tt("dve", comb, goh.unsqueeze(3).broadcast_to([128, 8, 4, 8]), wk.unsqueeze(2).broadcast_to([128, 8, 4, 8]), ALU.mult, R, R)
    if stop == "router":
        dump("comb", flat(comb), RB); dump("lg", flat(lg), RB)
        return finish(nc, tk, es, dsems)
    tk.barrier()
    ar.free_group("xp")
    ar.group("moe2")
    g2bc = ar.alloc(2048, F32); g2B = Buf("g2bc")
    tmpb = ar.alloc(128, F32); tmpbB = Buf("tmpb2"); tmpb_hi = ar.alloc(128, BF16); tmpb_lo = ar.alloc(128, BF16)
    row_bcast(g2bc, g2B, g2f)
    NRING = 4
    ring = [ar.alloc(16 * 512, BF16) for _ in range(NRING)]; ringB = [Buf("ring%d" % j) for j in range(NRING)]
    rn = {"n": 0}
    w2st = [ar.alloc(1024, F32) for _ in range(2)]; w2stB = [Buf("w2s0"), Buf("w2s1")]
    w2n = {"n": 0}
    heT = ar.alloc(4 * 1024, BF16, shape=(4,)); heB = [Buf("he%d" % f) for f in range(4)]
    sg = [ar.alloc(512, F32) for _ in range(2)]; sgB = [Buf("sg0"), Buf("sg1")]

    def ring_next():
        j = rn["n"] % NRING
        rn["n"] += 1
        return ring[j], ringB[j], j

    def load_up(dram_e):
        r_, rb_, j = ring_next()
        r3 = r_.rearrange("p (k n) -> p k n", k=16)
        dma("pool", r3, dram_e.rearrange("(k p) n -> p k n", p=128), [], [rb_], "ring%d" % j)
        return r3, rb_

    def load_w2(e):
        r_, rb_, j = ring_next()
        r3 = r_.rearrange("p (f n) -> p f n", f=4)
        for f in range(4):
            for hh in range(2):
                sb = w2n["n"] % 2
                w2n["n"] += 1
                dma("sp", w2st[sb], w2(e)[f * 128:(f + 1) * 128, hh * 1024:(hh + 1) * 1024], [], [w2stB[sb]], "w2s%d" % sb)
                tt("pool", r3[:, f, hh * 1024:(hh + 1) * 1024], w2st[sb], g2bc[:, hh * 1024:(hh + 1) * 1024], ALU.mult, [w2stB[sb], g2B], [rb_])
        return r3, rb_

    NEX = NE if stop == "end" else 1
    nxt = (load_up(w1(0)), load_up(w3(0)), load_w2(0))
    cnt_ev = 0
    for e in range(NEX):
        (W1, W1B), (W3, W3B), (W2, W2B) = nxt
        ge, ee = e // 8, e % 8
        for f in range(4):
            for h in range(2):
                pa, pb_ = (f * 2 + h) % 2, 2 + (f * 2 + h) % 2
                for kc in range(NKC):
                    mm(bank(pa), W1[:, kc, f * 128:(f + 1) * 128], fnT[:, kc, h * 512:(h + 1) * 512], kc == 0, kc == NKC - 1,
                       [W1B] + fnB[4 * h:4 * h + 4], [PB[pa]])
                for kc in range(NKC):
                    mm(bank(pb_), W3[:, kc, f * 128:(f + 1) * 128], fnT[:, kc, h * 512:(h + 1) * 512], kc == 0, kc == NKC - 1,
                       [W3B] + fnB[4 * h:4 * h + 4], [PB[pb_]])
                si = (f * 2 + h) % 2
                act(sg[si], bank(pa), AF.Silu, [PB[pa]], [sgB[si]])
                tt("dve", heT[:, f, h * 512:(h + 1) * 512], sg[si], bank(pb_), ALU.mult, [sgB[si], PB[pb_]], [heB[f]])
            if f == 1 and e + 1 < NEX:
                nxt1 = load_up(w1(e + 1))
        if e + 1 < NEX:
            nxt3 = load_up(w3(e + 1))
        for i in range(NT):
            for dq4 in range(4):
                pd = 4 + (i * 4 + dq4) % 3
                for f in range(4):
                    mm(bank(pd), heT[:, f, i * 128:(i + 1) * 128], W2[:, f, dq4 * 512:(dq4 + 1) * 512], f == 0, f == 3, [heB[f], W2B], [PB[pd]])
                stt(h_lat[:, i, dq4 * 512:(dq4 + 1) * 512], bank(pd), comb[:, i, ge, ee:ee + 1], h_lat[:, i, dq4 * 512:(dq4 + 1) * 512],
                    ALU.mult, ALU.add, [PB[pd], RB, hlB[i]], [hlB[i]])
        if e + 1 < NEX:
            nxt = (nxt1, nxt3, load_w2(e + 1))
    if stop == "moe1":
        dump("hl0", h_lat[:, 0, :], hlB[0])
        return finish(nc, tk, es, dsems)
    tk.barrier()
    ar.free_group("moe2")

    ar.group("fin")
    fgb = ar.alloc(2048, F32); fgB = Buf("fg")
    dma("sp", fgb, fg_bc, [], [fgB], "fg")
    fss = [ar.alloc(32, F32) for _ in range(2)]; fssB = [Buf("fss0"), Buf("fss1")]
    eps2 = ar.alloc(8, F32)
    memset("pool", eps2, EPS, [CB])
    ost = [ar.alloc(2048, F32) for _ in range(2)]; ostB = [Buf("ost0"), Buf("ost1")]
    for i in range(NT):
        j = i % 2
        ss = fss[j]
        for q4 in range(4):
            tk.op("dve", lambda e_, q4=q4, ss=ss: e_.bn_stats(out=ss[:, 8 + q4 * 6:8 + (q4 + 1) * 6], in_=h_lat[:, i, q4 * 512:(q4 + 1) * 512]), [hlB[i]], [fssB[j]])
        tk.op("dve", lambda e_, ss=ss: e_.bn_aggr(out=ss[:, 0:2], in_=ss[:, 8:32]), [fssB[j]], [fssB[j]])
        stt(ss[:, 2:3], ss[:, 0:1], ss[:, 0:1], ss[:, 1:2], ALU.mult, ALU.add, [fssB[j]], [fssB[j]])
        act(ss[:, 4:5], ss[:, 2:3], AF.Sqrt, [fssB[j], CB], [fssB[j]], bias=eps2[:, 0:1])
        tk.op("dve", lambda e_, ss=ss: e_.reciprocal(out=ss[:, 3:4], in_=ss[:, 4:5]), [fssB[j]], [fssB[j]])
        stt(ost[j], h_lat[:, i, :], ss[:, 3:4], fgb, ALU.mult, ALU.mult, [hlB[i], fssB[j], fgB], [ostB[j]])
        dma("pool", out[i * 128:(i + 1) * 128, :], ost[j], [ostB[j]], [], "ost%d" % j)
    return finish(nc, tk, es, dsems)


def finish(nc, tk, es, dsems):
    tk.final_wait("sp", list(dsems.keys()))
    tk.barrier()
    nc.all_engine_barrier()
    for h in tk.all_sems():
        nc.gpsimd.sem_clear(h)
    nc.all_engine_barrier()
    es.close()
    return nc


def build_moe():
    stop = "end"
    nc = bass.Bass("TRN2", target_bir_lowering=False)

    def din(name, shape):
        return nc.dram_tensor(name, list(shape), F32, kind="ExternalInput").ap()

    hl_in = din("hl_in", [TOK, D])
    modv_in = din("modv_in", [128, 48])
    wgr_fm = din("wgr_fm", [128, 16 * 36])
    bgr_bc = din("bgr_bc", [128, 36])
    NEd = NE
    NSPL = 4
    EPS_ = NEd // NSPL
    w1s = [din("w1_%d" % j, [EPS_, D, DE]) for j in range(NSPL)]
    w3s = [din("w3_%d" % j, [EPS_, D, DE]) for j in range(NSPL)]
    w2s = [din("w2_%d" % j, [EPS_, DE, D]) for j in range(NSPL)]
    w1 = lambda e: w1s[e // EPS_][e % EPS_]
    w3 = lambda e: w3s[e // EPS_][e % EPS_]
    w2 = lambda e: w2s[e // EPS_][e % EPS_]
    fg_bc = din("fg_bc", [128, D])
    out = nc.dram_tensor("out", [TOK, D], F32, kind="ExternalOutput").ap()
    dump_aps = {}
    tk = Tk(nc)
    nc.all_engine_barrier()
    for h in tk.all_sems():
        nc.gpsimd.sem_clear(h)
    nc.all_engine_barrier()
    NW = 53200
    import contextlib
    es = contextlib.ExitStack()
    arena_t = es.enter_context(nc.sbuf_tensor("arena", [128, NW], F32))
    ps_t = es.enter_context(nc.psum_tensor("ps", [128, 4096], F32))
    ar = Arena(arena_t, NW)
    dq = {"n": 0}

    def act(out_, in_, func, reads, writes, bias=None, scale=None, accum=None):
        kw = {}
        if bias is not None:
            kw["bias"] = bias
        if scale is not None:
            kw["scale"] = scale
        if accum is not None:
            kw["accum_out"] = accum
        return tk.op("act", lambda e: e.activation(out=out_, in_=in_, func=func, **kw), reads, writes)

    def ts(eng, out_, in0, s1, s2, op0, op1, reads, writes):
        if op1 is None:
            return tk.op(eng, lambda e: e.tensor_scalar(out=out_, in0=in0, scalar1=s1, scalar2=None, op0=op0), reads, writes)
        return tk.op(eng, lambda e: e.tensor_scalar(out=out_, in0=in0, scalar1=s1, scalar2=s2, op0=op0, op1=op1), reads, writes)

    def tt(eng, out_, in0, in1, op, reads, writes):
        return tk.op(eng, lambda e: e.tensor_tensor(out=out_, in0=in0, in1=in1, op=op), reads, writes)

    def stt(out_, in0, scalar, in1, op0, op1, reads, writes):
        return tk.op("dve", lambda e: e.scalar_tensor_tensor(out=out_, in0=in0, scalar=scalar, in1=in1, op0=op0, op1=op1), reads, writes)

    def cp(eng, out_, in_, reads, writes):
        if eng == "act":
            return act(out_, in_, AF.Copy, reads, writes)
        return tk.op(eng, lambda e: e.tensor_copy(out=out_, in_=in_), reads, writes)

    def mm(out_, lhsT, rhs, start, stop, reads, writes, sig=None):
        if sig is None:
            sig = stop
        return tk.op("pe", lambda e: e.matmul(out_, lhsT=lhsT, rhs=rhs, start=start, stop=stop), reads, writes, sig=sig)

    def trp(out_, in_, ident, reads, writes, sig=True):
        return tk.op("pe", lambda e: e.transpose(out=out_, in_=in_, identity=ident), reads, writes, sig=sig)

    dsems = {}

    def dma(q, out_, in_, reads, writes, sem):
        if sem not in dsems:
            dsems[sem] = tk.dsem(sem)
        return tk.op(q, lambda e: e.dma_start(out=out_, in_=in_), reads, writes, dsem=sem)

    def memset(eng, ap, val, writes):
        return tk.op(eng, lambda e: e.memset(ap, val), (), writes)

    def dump(nm, ap, buf):
        if nm in dump_aps:
            dma("pool", dump_aps[nm], ap, [buf], [], "dump")

    def bank(i):
        return ps_t[:, i * 512:(i + 1) * 512]

    PB = [Buf("pb%d" % i) for i in range(8)]
    P7a = ps_t[:, 7 * 512:7 * 512 + 128].bitcast(BF16)
    P7 = [ps_t[:, 7 * 512 + 256 + 32 * j:7 * 512 + 256 + 32 * (j + 1)] for j in range(8)]
    P7B = [Buf("p7_%d" % j) for j in range(8)]

    ident_bf = ar.alloc(128, BF16)
    ident_f = ar.alloc(128, F32)
    Umask = ar.alloc(128, F32)
    Lmask = ar.alloc(128, F32)
    ones_f = ar.alloc(128, F32)
    ones_bf = ar.alloc(128, BF16)
    CB = Buf("consts")
    memset("pool", ones_f, 1.0, [CB])
    memset("pool", ones_bf, 1.0, [CB])
    tk.op("pool", lambda e: e.affine_select(out=ident_f, in_=ones_f, pattern=[[-1, 128]], compare_op=ALU.is_equal, fill=0.0, base=0, channel_multiplier=1), [CB], [CB])
    tk.op("pool", lambda e: e.affine_select(out=ident_bf, in_=ones_bf, pattern=[[-1, 128]], compare_op=ALU.is_equal, fill=0.0, base=0, channel_multiplier=1), [CB], [CB])
    tk.op("pool", lambda e: e.affine_select(out=Umask, in_=ones_f, pattern=[[1, 128]], compare_op=ALU.is_ge, fill=0.0, base=0, channel_multiplier=-1), [CB], [CB])
    tk.op("pool", lambda e: e.affine_select(out=Lmask, in_=ones_f, pattern=[[-1, 128]], compare_op=ALU.is_ge, fill=0.0, base=0, channel_multiplier=1), [CB], [CB])

    U_bf = ar.alloc(128, BF16)
    L_bf = ar.alloc(128, BF16)
    cp("pool", U_bf, Umask, [CB], [CB])
    cp("pool", L_bf, Lmask, [CB], [CB])

    def load_const(dram, n, q="sp"):
        t = ar.alloc(n, F32)
        dma(q, t, dram, [], [CB], "consts")
        return t


    bgr_sb = load_const(bgr_bc, 36)
    wgr_bf = ar.alloc(16 * 36, BF16, shape=(16,))
    dma("pool", wgr_bf, wgr_fm.rearrange("p (k n) -> p k n", k=16), [], [CB], "consts")
    modv_sb = load_const(modv_in, 48)
    gm2 = modv_sb[:, 0:16]
    sh2 = modv_sb[:, 16:32]
    g2f = modv_sb[:, 32:48]
    ar.group("xp")
    xp_ss = [ar.alloc(32, F32) for _ in range(2)]
    eps_c = ar.alloc(8, F32)
    memset("pool", eps_c, EPS, [CB])
    xp_xb = [ar.alloc(D, BF16) for _ in range(2)]
    xp_b = [Buf("xp0"), Buf("xp1")]
    xpc = {"n": 0}
    P7aB = [Buf("p7a0"), Buf("p7a1")]
    P7a_v = [ps_t[:, 7 * 512 + 128 * j:7 * 512 + 128 * (j + 1)].bitcast(BF16) for j in range(2)]

    def xpath(src, srcb, dst, dstb, gmc, shc):
        i = xpc["n"] % 2
        xpc["n"] += 1
        ss, xb, xb_b = xp_ss[i], xp_xb[i], xp_b[i]
        for q4 in range(4):
            tk.op("dve", lambda e, q4=q4: e.bn_stats(out=ss[:, 8 + q4 * 6:8 + (q4 + 1) * 6], in_=src[:, q4 * 512:(q4 + 1) * 512]), [srcb], [xb_b])
        tk.op("dve", lambda e: e.bn_aggr(out=ss[:, 0:2], in_=ss[:, 8:32]), [xb_b], [xb_b])
        stt(ss[:, 2:3], ss[:, 0:1], ss[:, 0:1], ss[:, 1:2], ALU.mult, ALU.add, [xb_b], [xb_b])
        act(ss[:, 4:5], ss[:, 2:3], AF.Sqrt, [xb_b], [xb_b], bias=eps_c[:, 0:1])
        tk.op("dve", lambda e: e.reciprocal(out=ss[:, 3:4], in_=ss[:, 4:5]), [xb_b], [xb_b])
        act(xb, src, AF.Copy, [srcb, xb_b], [xb_b], scale=ss[:, 3:4])
        for g in range(8):
            pj = g % 2
            for k2 in range(2):
                kc = g * 2 + k2
                trp(P7a_v[pj][:, k2 * 128:(k2 + 1) * 128], xb[:, kc * 128:(kc + 1) * 128], ident_bf, [xb_b, CB], [P7aB[pj]], sig=(k2 == 1))
            for k2 in range(2):
                kc = g * 2 + k2
                if kc % 2 == 0:
                    ts("dve", dst(kc), P7a_v[pj][:, k2 * 128:(k2 + 1) * 128], gmc(kc), shc(kc), ALU.mult, ALU.add, [P7aB[pj], CB], [dstb])
                else:
                    act(dst(kc), P7a_v[pj][:, k2 * 128:(k2 + 1) * 128], AF.Identity, [P7aB[pj], CB], [dstb], bias=shc(kc), scale=gmc(kc))


    ar.group("base")

    def flat(ap):
        n = len(ap.shape)
        if n == 3:
            return ap.rearrange("p a b -> p (a b)")
        if n == 4:
            return ap.rearrange("p a b c -> p (a b c)")
        return ap

    def row_bcast(dst, dstB, vec_fm):
        for kc in range(NKC):
            ts("dve", tmpb, ones_f, vec_fm[:, kc:kc + 1], None, ALU.mult, None, [CB, tmpbB], [tmpbB])
            cp("dve", tmpb_hi, tmpb, [tmpbB], [tmpbB])
            tt("dve", tmpb, tmpb, tmpb_hi, ALU.subtract, [tmpbB], [tmpbB])
            cp("dve", tmpb_lo, tmpb, [tmpbB], [tmpbB])
            pb = kc % 2
            mm(bank(pb)[:, 0:128], tmpb_hi, ident_bf, True, False, [tmpbB, CB], [PB[pb]], sig=False)
            mm(bank(pb)[:, 0:128], tmpb_lo, ident_bf, False, True, [tmpbB, CB], [PB[pb]])
            cp("act", dst[:, kc * 128:(kc + 1) * 128], bank(pb)[:, 0:128], [PB[pb]], [dstB])

    ar.group("hlat")
    h_lat = ar.alloc(8 * 2048, F32, shape=(8,)); hlB = [Buf("hl%d" % i) for i in range(8)]
    for i in range(NT):
        dma("sp", h_lat[:, i, :], hl_in[i * 128:(i + 1) * 128, :], [], [hlB[i]], "hl%d" % i)
    ar.group("xp")
    xp_ss = [ar.alloc(32, F32) for _ in range(2)]
    eps_c = ar.alloc(8, F32)
    memset("pool", eps_c, EPS, [CB])
    xp_xb = [ar.alloc(D, BF16) for _ in range(2)]
    xp_b = [Buf("xq0"), Buf("xq1")]
    ar.group("moe")
    fnT = ar.alloc(16 * 1024, BF16, shape=(16,)); fnB = [Buf("fn%d" % i) for i in range(8)]
    gm2c = lambda kc: gm2[:, kc:kc + 1]
    sh2c = lambda kc: sh2[:, kc:kc + 1]
    for i in range(NT):
        xpath(h_lat[:, i, :], hlB[i], lambda kc, i=i: fnT[:, kc, i * 128:(i + 1) * 128], fnB[i], gm2c, sh2c)
    lg = ar.alloc(8 * 36, F32, shape=(8,)); RB = Buf("router")
    for i in range(NT):
        for kc in range(NKC):
            mm(bank(0)[:, 0:36], fnT[:, kc, i * 128:(i + 1) * 128], wgr_bf[:, kc, :], kc == 0, kc == NKC - 1, [fnB[i], CB], [PB[0]])
        tt("dve", lg[:, i, :], bank(0)[:, 0:36], bgr_sb, ALU.add, [PB[0], CB], [RB])
    comb = ar.alloc(8 * 32, F32, shape=(8, 4))
    gmx = ar.alloc(8, F32); goh = ar.alloc(32, F32, shape=(8,)); gex = ar.alloc(32, F32, shape=(8,)); gsum = ar.alloc(8, F32); pg = ar.alloc(8, F32)
    selg = ar.alloc(64, F32, shape=(8,)); tmp32 = ar.alloc(256, F32, shape=(8, 4))
    m1 = ar.alloc(8, F32); m2 = ar.alloc(8, F32); k1 = ar.alloc(64, F32, shape=(8,)); k2 = ar.alloc(64, F32, shape=(8,)); sel2 = ar.alloc(64, F32, shape=(8,))
    p1 = ar.alloc(8, F32); p2 = ar.alloc(8, F32); wk = ar.alloc(64, F32, shape=(8,))
    gl = lg[:, :, 0:4]
    el = lg[:, :, 4:36].rearrange("p t (g e) -> p t g e", g=4)
    R = [RB]
    tk.op("dve", lambda e: e.tensor_reduce(out=gmx, in_=gl, axis=AX.X, op=ALU.max), R, R)
    bc = lambda ap, shp: ap.broadcast_to(shp)
    tt("dve", goh, gl, gmx.unsqueeze(2).broadcast_to([128, 8, 4]), ALU.is_equal, R, R)
    tt("dve", gex, gl, gmx.unsqueeze(2).broadcast_to([128, 8, 4]), ALU.subtract, R, R)
    act(gex, gex, AF.Exp, R, R)
    tk.op("dve", lambda e: e.tensor_reduce(out=gsum, in_=gex, axis=AX.X, op=ALU.add), R, R)
    tk.op("dve", lambda e: e.reciprocal(out=pg, in_=gsum), R, R)
    tt("dve", tmp32, el, goh.unsqueeze(3).broadcast_to([128, 8, 4, 8]), ALU.mult, R, R)
    tk.op("dve", lambda e: e.tensor_reduce(out=selg, in_=tmp32.rearrange("p t g e -> p t e g"), axis=AX.X, op=ALU.add), R, R)
    tk.op("dve", lambda e: e.tensor_reduce(out=m1, in_=selg, axis=AX.X, op=ALU.max), R, R)
    tt("dve", k1, selg, m1.unsqueeze(2).broadcast_to([128, 8, 8]), ALU.is_equal, R, R)
    stt(sel2, k1, -1e30, selg, ALU.mult, ALU.add, R, R)
    tk.op("dve", lambda e: e.tensor_reduce(out=m2, in_=sel2, axis=AX.X, op=ALU.max), R, R)
    tt("dve", k2, sel2, m2.unsqueeze(2).broadcast_to([128, 8, 8]), ALU.is_equal, R, R)
    tt("dve", p1, m1, m2, ALU.subtract, R, R)
    act(p1, p1, AF.Sigmoid, R, R)
    ts("dve", p2, p1, -1.0, 1.0, ALU.mult, ALU.add, R, R)
    tt("dve", p1, p1, pg, ALU.mult, R, R)
    tt("dve", p2, p2, pg, ALU.mult, R, R)
    tt("dve", wk, k1, p1.unsqueeze(2).broadcast_to([128, 8, 8]), ALU.mult, R, R)
    tt("dve", k2, k2, p2.unsqueeze(2).broadcast_to([128, 8, 8]), ALU.mult, R, R)
    tt("dve", wk, wk, k2, ALU.add, R, R)
    tt("dve", comb, goh.unsqueeze(3).broadcast_to([128, 8, 4, 8]), wk.unsqueeze(2).broadcast_to([128, 8, 4, 8]), ALU.mult, R, R)
    if stop == "router":
        dump("comb", flat(comb), RB); dump("lg", flat(lg), RB)
        return finish(nc, tk, es, dsems)
    tk.barrier()
    ar.free_group("xp")
    ar.group("moe2")
    g2bc = ar.alloc(2048, F32); g2B = Buf("g2bc")
    tmpb = ar.alloc(128, F32); tmpbB = Buf("tmpb2"); tmpb_hi = ar.alloc(128, BF16); tmpb_lo = ar.alloc(128, BF16)
    row_bcast(g2bc, g2B, g2f)
    NRING = 4
    ring = [ar.alloc(16 * 512, BF16) for _ in range(NRING)]; ringB = [Buf("ring%d" % j) for j in range(NRING)]
    rn = {"n": 0}
    w2st = [ar.alloc(1024, F32) for _ in range(2)]; w2stB = [Buf("w2s0"), Buf("w2s1")]
    w2n = {"n": 0}
    heT = ar.alloc(4 * 1024, BF16, shape=(4,)); heB = [Buf("he%d" % f) for f in range(4)]
    sg = [ar.alloc(512, F32) for _ in range(2)]; sgB = [Buf("sg0"), Buf("sg1")]

    def ring_next():
        j = rn["n"] % NRING
        rn["n"] += 1
        return ring[j], ringB[j], j

    def load_up(dram_e):
        r_, rb_, j = ring_next()
        r3 = r_.rearrange("p (k n) -> p k n", k=16)
        dma("pool", r3, dram_e.rearrange("(k p) n -> p k n", p=128), [], [rb_], "ring%d" % j)
        return r3, rb_

    def load_w2(e):
        r_, rb_, j = ring_next()
        r3 = r_.rearrange("p (f n) -> p f n", f=4)
        for f in range(4):
            for hh in range(2):
                sb = w2n["n"] % 2
                w2n["n"] += 1
                dma("sp", w2st[sb], w2(e)[f * 128:(f + 1) * 128, hh * 1024:(hh + 1) * 1024], [], [w2stB[sb]], "w2s%d" % sb)
                tt("pool", r3[:, f, hh * 1024:(hh + 1) * 1024], w2st[sb], g2bc[:, hh * 1024:(hh + 1) * 1024], ALU.mult, [w2stB[sb], g2B], [rb_])
        return r3, rb_

    NEX = NE if stop == "end" else 1
    nxt = (load_up(w1(0)), load_up(w3(0)), load_w2(0))
    cnt_ev = 0
    for e in range(NEX):
        (W1, W1B), (W3, W3B), (W2, W2B) = nxt
        ge, ee = e // 8, e % 8
        for f in range(4):
            for h in range(2):
                pa, pb_ = (f * 2 + h) % 2, 2 + (f * 2 + h) % 2
                for kc in range(NKC):
                    mm(bank(pa), W1[:, kc, f * 128:(f + 1) * 128], fnT[:, kc, h * 512:(h + 1) * 512], kc == 0, kc == NKC - 1,
                       [W1B] + fnB[4 * h:4 * h + 4], [PB[pa]])
                for kc in range(NKC):
                    mm(bank(pb_), W3[:, kc, f * 128:(f + 1) * 128], fnT[:, kc, h * 512:(h + 1) * 512], kc == 0, kc == NKC - 1,
                       [W3B] + fnB[4 * h:4 * h + 4], [PB[pb_]])
                si = (f * 2 + h) % 2
                act(sg[si], bank(pa), AF.Silu, [PB[pa]], [sgB[si]])
                tt("dve", heT[:, f, h * 512:(h + 1) * 512], sg[si], bank(pb_), ALU.mult, [sgB[si], PB[pb_]], [heB[f]])
            if f == 1 and e + 1 < NEX:
                nxt1 = load_up(w1(e + 1))
        if e + 1 < NEX:
            nxt3 = load_up(w3(e + 1))
        for i in range(NT):
            for dq4 in range(4):
                pd = 4 + (i * 4 + dq4) % 3
                for f in range(4):
                    mm(bank(pd), heT[:, f, i * 128:(i + 1) * 128], W2[:, f, dq4 * 512:(dq4 + 1) * 512], f == 0, f == 3, [heB[f], W2B], [PB[pd]])
                stt(h_lat[:, i, dq4 * 512:(dq4 + 1) * 512], bank(pd), comb[:, i, ge, ee:ee + 1], h_lat[:, i, dq4 * 512:(dq4 + 1) * 512],
                    ALU.mult, ALU.add, [PB[pd], RB, hlB[i]], [hlB[i]])
        if e + 1 < NEX:
            nxt = (nxt1, nxt3, load_w2(e + 1))
    if stop == "moe1":
        dump("hl0", h_lat[:, 0, :], hlB[0])
        return finish(nc, tk, es, dsems)
    tk.barrier()
    ar.free_group("moe2")

    ar.group("fin")
    fgb = ar.alloc(2048, F32); fgB = Buf("fg")
    dma("sp", fgb, fg_bc, [], [fgB], "fg")
    fss = [ar.alloc(32, F32) for _ in range(2)]; fssB = [Buf("fss0"), Buf("fss1")]
    eps2 = ar.alloc(8, F32)
    memset("pool", eps2, EPS, [CB])
    ost = [ar.alloc(2048, F32) for _ in range(2)]; ostB = [Buf("ost0"), Buf("ost1")]
    for i in range(NT):
        j = i % 2
        ss = fss[j]
        for q4 in range(4):
            tk.op("dve", lambda e_, q4=q4, ss=ss: e_.bn_stats(out=ss[:, 8 + q4 * 6:8 + (q4 + 1) * 6], in_=h_lat[:, i, q4 * 512:(q4 + 1) * 512]), [hlB[i]], [fssB[j]])
        tk.op("dve", lambda e_, ss=ss: e_.bn_aggr(out=ss[:, 0:2], in_=ss[:, 8:32]), [fssB[j]], [fssB[j]])
        stt(ss[:, 2:3], ss[:, 0:1], ss[:, 0:1], ss[:, 1:2], ALU.mult, ALU.add, [fssB[j]], [fssB[j]])
        act(ss[:, 4:5], ss[:, 2:3], AF.Sqrt, [fssB[j], CB], [fssB[j]], bias=eps2[:, 0:1])
        tk.op("dve", lambda e_, ss=ss: e_.reciprocal(out=ss[:, 3:4], in_=ss[:, 4:5]), [fssB[j]], [fssB[j]])
        stt(ost[j], h_lat[:, i, :], ss[:, 3:4], fgb, ALU.mult, ALU.mult, [hlB[i], fssB[j], fgB], [ostB[j]])
        dma("pool", out[i * 128:(i + 1) * 128, :], ost[j], [ostB[j]], [], "ost%d" % j)

    return finish(nc, tk, es, dsems)


def _fm(v, n):
    return np.ascontiguousarray(np.asarray(v, np.float32).reshape(n, 128).T)


def _bc(v):
    v = np.asarray(v, np.float32).reshape(1, -1)
    return np.ascontiguousarray(np.broadcast_to(v, (128, v.shape[1])))


def prep_inputs(x, c, ctx, c_ctx, w_mod, b_mod, norm1_g, w_in, w_conv_q, w_conv_k, gate_bias,
                head_norm_g, w_pool, pool_scale, w_out, norm2_g, w_group, b_group, w_router,
                b_router, w1, w3, w2, final_g):
    f32 = lambda a: np.ascontiguousarray(np.asarray(a, np.float32))
    x2 = f32(x)[0]
    ctx2 = f32(ctx)[0]
    w_in2 = f32(w_in)[0]
    sh = {}
    sh["w_mod"] = f32(w_mod)[0]
    bm = _fm(f32(b_mod)[0], 96)
    sh["bmod_fm"] = np.ascontiguousarray(np.repeat(bm[:, :, None], 2, axis=2).reshape(128, 192))
    cf = np.stack([_fm(f32(c)[0], 16), _fm(f32(c_ctx), 16)], axis=2)
    sh["c_fm"] = np.ascontiguousarray(cf.reshape(128, 32))
    n1 = _fm(f32(norm1_g)[0], 16)
    sh["n1g_fm"] = np.ascontiguousarray(np.repeat(n1[:, :, None], 2, axis=2).reshape(128, 32))
    sh["n2g_fm"] = _fm(f32(norm2_g)[0], 16)
    sh["w_in"] = w_in2
    wq = f32(w_conv_q)[0]
    wk = f32(w_conv_k)[0]
    tap_fm = lambda w: np.ascontiguousarray(w.reshape(3, 8, 128).transpose(2, 1, 0).reshape(128, 24))
    sh["wcq_fm"] = tap_fm(wq)
    sh["gbias_bc"] = _bc(f32(gate_bias)[0].reshape(-1))
    sh["hng_fm"] = _fm(f32(head_norm_g)[0], 8)
    sh["pscale_fm"] = _fm(f32(pool_scale)[0], 8)
    sh["w_pool"] = f32(w_pool)[0]
    sh["w_out"] = f32(w_out)[0]
    wgr = np.concatenate([f32(w_group)[0], f32(w_router)[0]], axis=1)
    sh["wgr_fm"] = np.ascontiguousarray(wgr.reshape(16, 128, 36).transpose(1, 0, 2).reshape(128, 16 * 36))
    sh["bgr_bc"] = _bc(np.concatenate([f32(b_group)[0], f32(b_router)[0]]))
    for j in range(4):
        sh["w1_%d" % j] = f32(w1)[0][j * 8:(j + 1) * 8]
        sh["w3_%d" % j] = f32(w3)[0][j * 8:(j + 1) * 8]
        sh["w2_%d" % j] = f32(w2)[0][j * 8:(j + 1) * 8]
    sh["fg_bc"] = _bc(f32(final_g))
    sh["ctxf"] = ctx2
    sh["ctxb"] = np.ascontiguousarray(ctx2[::-1])
    gb = f32(gate_bias)[0].reshape(-1)
    wgF = w_in2[:, G0:G0 + 8]
    wgB = w_in2[:, G0 + 8:G0 + 16]
    tapF = tap_fm(wk)
    tapB = tap_fm(wk[::-1])
    zrow = np.zeros((D,), np.float32)
    maps = []
    for cc in range(NCORES):
        m = dict(sh)
        T0 = cc * TOK
        m["xown"] = np.ascontiguousarray(x2[T0:T0 + TOK])
        halo = np.zeros((TOK, D), np.float32)
        hm = np.zeros((2048,), np.float32)
        hm[512:1536] = 1.0
        if cc > 0:
            halo[0:512] = x2[T0 - 512:T0]
            hm[0:512] = 1.0
        if cc < NCORES - 1:
            halo[512:1024] = x2[T0 + TOK:T0 + TOK + 512]
            hm[1536:2048] = 1.0
        m["xhalo"] = halo
        m["hv"] = _bc(np.array([hm[0], hm[2047]], np.float32))
        xo_ = np.empty((7 * TOK, D), np.float32)
        xe_ = np.zeros((128, D), np.float32)
        em = np.zeros((128,), np.float32)
        wgs = np.zeros((9, D, 8), np.float32)
        bgs = np.zeros((9, 8), np.float32)
        taps = np.zeros((11, 128, 24), np.float32)
        wgs[0], bgs[0], taps[0] = wgF, gb[0:8], tapF
        wgs[1], bgs[1], taps[1] = wgB, gb[8:16], tapB
        taps[9] = tapF
        taps[10] = tap_fm(wq)
        for j in range(7):
            if j < cc:
                s = j
                seg = x2[s * TOK:(s + 1) * TOK]
                prev = x2[s * TOK - 1] if s > 0 else None
                nxt = x2[(s + 1) * TOK]
                wgs[2 + j], bgs[2 + j], taps[2 + j] = wgF, gb[0:8], tapF
            else:
                s = 7 - (j - cc)
                seg = x2[s * TOK:(s + 1) * TOK][::-1]
                prev = x2[(s + 1) * TOK] if s < 7 else None
                nxt = x2[s * TOK - 1]
                wgs[2 + j], bgs[2 + j], taps[2 + j] = wgB, gb[8:16], tapB
            xo_[j * TOK:(j + 1) * TOK] = seg
            if prev is not None:
                xe_[2 * j] = prev
                em[2 * j] = 1.0
            xe_[2 * j + 1] = nxt
            em[2 * j + 1] = 1.0
        m["xo"] = xo_
        m["xe"] = xe_
        m["emask"] = _bc(em)
        m["wgs_fm"] = np.ascontiguousarray(wgs.reshape(9, 16, 128, 8).transpose(2, 0, 1, 3).reshape(128, 9 * 16 * 8))
        m["bg_s"] = _bc(bgs.reshape(-1))
        m["taps_s"] = np.ascontiguousarray(taps.transpose(1, 0, 2).reshape(128, 264))
        sel = np.zeros((8, 2), np.float32)
        sel[:, 1] = 1.0
        sel[cc, 0] = 1.0
        sel[cc, 1] = 0.0
        m["sel"] = _bc(sel.reshape(-1))
        inv = np.zeros((4, 16, 64), np.float32)
        for gi, w in enumerate(WINS):
            r = np.arange(128)
            cnt_r = np.clip(r - w // 2 + w, 0, 128) - np.clip(r - w // 2, 0, 128)
            cl = np.arange(64)
            cnt_c = np.clip(cl - w // 2 + w, 0, 64) - np.clip(cl - w // 2, 0, 64)
            inv[gi] = 1.0 / (cnt_r[16 * cc:16 * cc + 16, None] * cnt_c[None, :])
        m["invcnt"] = _bc(inv.reshape(-1))
        maps.append(m)
    return maps


_NC_CACHE = {}
FUSED = True


def kernel(**inputs):
    maps = prep_inputs(**inputs)
    if FUSED:
        if "nc" not in _NC_CACHE:
            _NC_CACHE["nc"] = build()
        res = run_bass_kernel_spmd(_NC_CACHE["nc"], maps, core_ids=list(range(NCORES)))
        outs = [np.asarray(r["out"], np.float32) for r in res.results]
        return np.concatenate(outs, axis=0).reshape(1, SEQ, D)
    if "nc1" not in _NC_CACHE:
        _NC_CACHE["nc1"] = build(stop="hlat_out")
        _NC_CACHE["nc2"] = build_moe()
    moe_keys = ["wgr_fm", "bgr_bc", "fg_bc"] + ["w%d_%d" % (a, j) for a in (1, 2, 3) for j in range(4)]
    maps1 = []
    for m in maps:
        m1 = {k: v for k, v in m.items() if k not in moe_keys or k in ("wgr_fm", "bgr_bc", "fg_bc")}
        for a in (1, 2, 3):
            m1["w%d_0" % a] = m["w%d_0" % a][:1]
        maps1.append(m1)
    res1 = run_bass_kernel_spmd(_NC_CACHE["nc1"], maps1, core_ids=list(range(NCORES)))
    maps2 = []
    for c, m in enumerate(maps):
        m2 = {k: m[k] for k in moe_keys}
        m2["hl_in"] = np.asarray(res1.results[c]["out"], np.float32)
        m2["modv_in"] = np.asarray(res1.results[c]["modv_out"], np.float32)
        maps2.append(m2)
    res = run_bass_kernel_spmd(_NC_CACHE["nc2"], maps2, core_ids=list(range(NCORES)))
    outs = [np.asarray(r["out"], np.float32) for r in res.results]
    return np.concatenate(outs, axis=0).reshape(1, SEQ, D)
```
